# Optimizing a Trainium2 kernel written in Bass

```python
import jax, jax.numpy as jnp
from jax import lax
import numpy as np

D_MODEL = 1024
BATCH = 8
SEQ = 2048
DEPTH = 2

HEAD_DIM = 64
RWKV_HEADS = 4
RWKV_WIDTH = RWKV_HEADS * HEAD_DIM
RWKV_DECAY_RANK = 32
RWKV_AAA_RANK = 32
RWKV_GATE_RANK = 64
RWKV_GN_EPS = 64e-5
RWKV_IN = 3 * RWKV_WIDTH + RWKV_DECAY_RANK + RWKV_AAA_RANK + RWKV_GATE_RANK
ATTN_GROUPS = ((128, 1), (512, 4), (2048, 16))
ATTN_HEADS_PER_GROUP = 2
ATTN_HEADS = len(ATTN_GROUPS) * ATTN_HEADS_PER_GROUP
ATTN_WIDTH = ATTN_HEADS * HEAD_DIM
ATTN_OUT_WIDTH = ATTN_HEADS_PER_GROUP * HEAD_DIM
ATTN_BLOCK = 128
ATTN_IN = 3 * ATTN_WIDTH
ROPE_THETA = 10000.0
MLSTM_HEADS = 4
MLSTM_WIDTH = MLSTM_HEADS * HEAD_DIM
MLSTM_CONV = 4
MLSTM_CHUNK = 64
MLSTM_IN = 4 * MLSTM_WIDTH + 2 * MLSTM_HEADS
HGRN_HEADS = 4
HGRN_WIDTH = HGRN_HEADS * HEAD_DIM
HGRN_CHUNK = 64
HGRN_IN = 4 * HGRN_WIDTH
N_BRANCHES = 4
GATE_IN = N_BRANCHES * D_MODEL
N_IN = RWKV_IN + ATTN_IN + MLSTM_IN + HGRN_IN + GATE_IN
D_FF = 2816
FFN_CONV = 3
NORM_EPS = 1e-6
NEG_INF = -1e30

kernel_name = 'hybrid_rwkv7_dilattn_mlstm_hgrn2_gated'


def split_cols(z, widths):
    return jnp.split(z, np.cumsum(widths)[:-1].tolist(), axis=-1)


def rmsnorm(x, g):
    xf = x.astype(jnp.float32)
    y = xf * lax.rsqrt(jnp.mean(xf * xf, axis=-1, keepdims=True) + NORM_EPS)
    return (y * g.astype(jnp.float32)).astype(x.dtype)


def token_shift(z):
    return jnp.pad(z, ((0, 0), (1, 0), (0, 0)))[:, :-1]


def causal_dwconv(x, w, b):
    K, C = w.shape
    y = lax.conv_general_dilated(x, w.astype(x.dtype)[:, None, :], window_strides=(1,),
                                 padding=[(K - 1, 0)], dimension_numbers=('NWC', 'WIO', 'NWC'),
                                 feature_group_count=C)
    return y + b.astype(x.dtype)


def rope(x, pos):
    half = x.shape[-1] // 2
    inv_freq = ROPE_THETA ** (-jnp.arange(half, dtype=jnp.float32) / half)
    ang = pos.astype(jnp.float32)[:, None] * inv_freq[None, :]
    cos = jnp.cos(ang)[None, :, None, :].astype(x.dtype)
    sin = jnp.sin(ang)[None, :, None, :].astype(x.dtype)
    x1, x2 = x[..., :half], x[..., half:]
    return jnp.concatenate([x1 * cos - x2 * sin, x1 * sin + x2 * cos], axis=-1)


def rwkv7_scan(r, w, k, v, a, b):
    Bsz, T, H, N = r.shape

    def step(S, inp):
        rt, wt, kt, vt, at, bt = inp
        sa = jnp.einsum('bhvk,bhk->bhv', S, at)
        S = S * wt[:, :, None, :] + sa[..., None] * bt[:, :, None, :] + vt[..., None] * kt[:, :, None, :]
        return S, jnp.einsum('bhvk,bhk->bhv', S, rt)

    xs = tuple(t.transpose(1, 0, 2, 3) for t in (r, w, k, v, a, b))
    _, y = lax.scan(step, jnp.zeros((Bsz, H, N, N), jnp.float32), xs)
    return y.transpose(1, 0, 2, 3)


def rwkv7_branch(z, mu, w0, w2, a0, a2, g2, k_k, k_a, r_k, ln_g, ln_b):
    Bsz, T, _ = z.shape
    f32 = jnp.float32
    z = z + mu * (token_shift(z) - z)
    r, k, v, wl, al, gl = split_cols(z, [RWKV_WIDTH, RWKV_WIDTH, RWKV_WIDTH,
                                         RWKV_DECAY_RANK, RWKV_AAA_RANK, RWKV_GATE_RANK])
    logw = -jax.nn.softplus(-(w0 + jnp.tanh(wl) @ w2).astype(f32)) - 0.5
    decay = jnp.exp(-jnp.exp(logw))
    a = jax.nn.sigmoid((a0 + al @ a2).astype(f32))
    g = jax.nn.sigmoid(gl) @ g2

    def heads(t):
        return t.astype(f32).reshape(Bsz, T, RWKV_HEADS, HEAD_DIM)

    kk = heads(k * k_k)
    kk = kk * lax.rsqrt(jnp.sum(kk * kk, axis=-1, keepdims=True) + 1e-12)
    k = k.astype(f32) * (1.0 + (a - 1.0) * k_a)
    rh, kh, vh, ah = heads(r), heads(k), heads(v), heads(a)
    y = rwkv7_scan(rh, heads(decay), kh, vh, -kk, kk * ah)
    mean = jnp.mean(y, axis=-1, keepdims=True)
    var = jnp.mean(jnp.square(y - mean), axis=-1, keepdims=True)
    y = ((y - mean) * lax.rsqrt(var + RWKV_GN_EPS)).reshape(Bsz, T, RWKV_WIDTH) * ln_g + ln_b
    bonus = jnp.sum(rh * kh * r_k, axis=-1, keepdims=True) * vh
    return (y + bonus.reshape(Bsz, T, RWKV_WIDTH)) * g


def dilated_window_attention(q, k, v, window, dilation):
    Bsz, T, H, Dh = q.shape
    L = T // dilation
    span = window // dilation
    nb = -(-L // ATTN_BLOCK)
    Lp = nb * ATTN_BLOCK

    def gather(t):
        t = t.reshape(Bsz, L, dilation, H, Dh).transpose(0, 2, 3, 1, 4)
        t = jnp.pad(t, ((0, 0), (0, 0), (0, 0), (0, Lp - L), (0, 0)))
        return t.reshape(Bsz, dilation, H, nb, ATTN_BLOCK, Dh)

    def with_prev(t):
        prev = jnp.pad(t, ((0, 0), (0, 0), (0, 0), (1, 0), (0, 0), (0, 0)))[:, :, :, :-1]
        return jnp.concatenate([prev, t], axis=4)

    qb = gather(q).astype(jnp.float32)
    kw = with_prev(gather(k)).astype(jnp.float32)
    vw = with_prev(gather(v)).astype(jnp.float32)
    qi = jnp.arange(ATTN_BLOCK)[:, None]
    kj = jnp.arange(2 * ATTN_BLOCK)[None, :]
    dist = qi - kj + ATTN_BLOCK
    band = (dist >= 0) & (dist <= span)
    key_step = jnp.arange(nb)[:, None, None] * ATTN_BLOCK + kj[None] - ATTN_BLOCK
    mask = band[None] & (key_step >= 0)
    s = jnp.einsum('bdhnqc,bdhnkc->bdhnqk', qb, kw)
    s = jnp.where(mask, s, NEG_INF)
    lse = jax.nn.logsumexp(s, axis=-1)
    p = jnp.exp(s - lse[..., None])
    o = jnp.einsum('bdhnqk,bdhnkc->bdhnqc', p, vw)
    o = o.reshape(Bsz, dilation, H, Lp, Dh)[:, :, :, :L].transpose(0, 3, 1, 2, 4).reshape(Bsz, T, H, Dh)
    lse = lse.reshape(Bsz, dilation, H, Lp)[:, :, :, :L].transpose(0, 3, 1, 2).reshape(Bsz, T, H)
    return o, lse


def dilated_attention_branch(z):
    Bsz, T, _ = z.shape
    q, k, v = split_cols(z, [ATTN_WIDTH, ATTN_WIDTH, ATTN_WIDTH])
    q = q.reshape(Bsz, T, ATTN_HEADS, HEAD_DIM)
    k = k.reshape(Bsz, T, ATTN_HEADS, HEAD_DIM)
    v = v.reshape(Bsz, T, ATTN_HEADS, HEAD_DIM)
    pos = jnp.arange(T)
    q = rope(q, pos) * (HEAD_DIM ** -0.5)
    k = rope(k, pos)
    outs, lses = [], []
    for gi, (win, dil) in enumerate(ATTN_GROUPS):
        sl = slice(gi * ATTN_HEADS_PER_GROUP, (gi + 1) * ATTN_HEADS_PER_GROUP)
        o, lse = dilated_window_attention(q[:, :, sl], k[:, :, sl], v[:, :, sl], win, dil)
        outs.append(o)
        lses.append(lse)
    alpha = jax.nn.softmax(jnp.stack(lses, axis=0), axis=0)
    o = jnp.sum(alpha[..., None] * jnp.stack(outs, axis=0), axis=0)
    return o.reshape(Bsz, T, ATTN_OUT_WIDTH)


def mlstm_chunkwise(q, k, v, i_pre, f_pre):
    Bsz, H, T, Dk = q.shape
    Dv = v.shape[-1]
    Lc = MLSTM_CHUNK
    nc = T // Lc
    lf = jax.nn.log_sigmoid(f_pre)

    def chunks(t):
        return jnp.moveaxis(t.reshape((Bsz, H, nc, Lc) + t.shape[3:]), 2, 0)

    causal = jnp.tril(jnp.ones((Lc, Lc), bool))

    def step(carry, inp):
        Cm, nv, m = carry
        qc, kc, vc, ic, fc = inp
        b = jnp.cumsum(fc, axis=-1)
        d_intra = jnp.where(causal, b[..., :, None] - b[..., None, :] + ic[..., None, :], -jnp.inf)
        d_inter = b + m[..., None]
        m_t = jnp.maximum(d_inter, jnp.max(d_intra, axis=-1))
        s = jnp.einsum('bhtd,bhsd->bhts', qc, kc) * jnp.exp(d_intra - m_t[..., None])
        w_inter = jnp.exp(d_inter - m_t)
        num = w_inter[..., None] * jnp.einsum('bhtd,bhdv->bhtv', qc, Cm) + jnp.einsum('bhts,bhsv->bhtv', s, vc)
        den = w_inter * jnp.einsum('bhtd,bhd->bht', qc, nv) + jnp.sum(s, axis=-1)
        h = num / jnp.maximum(jnp.abs(den), jnp.exp(-m_t))[..., None]
        b_end = b[..., -1]
        g_end = b_end[..., None] - b + ic
        m_new = jnp.maximum(b_end + m, jnp.max(g_end, axis=-1))
        w_k = jnp.exp(g_end - m_new[..., None])
        dec = jnp.exp(b_end + m - m_new)
        Cm = dec[..., None, None] * Cm + jnp.einsum('bhs,bhsd,bhsv->bhdv', w_k, kc, vc)
        nv = dec[..., None] * nv + jnp.einsum('bhs,bhsd->bhd', w_k, kc)
        return (Cm, nv, m_new), h

    init = (jnp.zeros((Bsz, H, Dk, Dv), jnp.float32), jnp.zeros((Bsz, H, Dk), jnp.float32),
            jnp.zeros((Bsz, H), jnp.float32))
    _, h = lax.scan(step, init, (chunks(q), chunks(k), chunks(v), chunks(i_pre), chunks(lf)))
    return jnp.moveaxis(h, 0, 2).reshape(Bsz, H, T, Dv)


def mlstm_branch(z, conv_w, conv_b, i_b, f_b):
    Bsz, T, _ = z.shape
    f32 = jnp.float32
    qk, v, o, ig, fg = split_cols(z, [2 * MLSTM_WIDTH, MLSTM_WIDTH, MLSTM_WIDTH, MLSTM_HEADS, MLSTM_HEADS])
    qk = jax.nn.silu(causal_dwconv(qk, conv_w, conv_b))
    q, k = split_cols(qk, [MLSTM_WIDTH, MLSTM_WIDTH])

    def heads(t):
        return t.astype(f32).reshape(Bsz, T, MLSTM_HEADS, HEAD_DIM).transpose(0, 2, 1, 3)

    i_pre = (ig + i_b).astype(f32).transpose(0, 2, 1)
    f_pre = (fg + f_b).astype(f32).transpose(0, 2, 1)
    h = mlstm_chunkwise(heads(q), heads(k) * (HEAD_DIM ** -0.5), heads(v), i_pre, f_pre)
    h = h.transpose(0, 2, 1, 3).reshape(Bsz, T, MLSTM_WIDTH)
    return jax.nn.sigmoid(o.astype(f32)) * h


def hgrn2_chunkwise(q, k, v, logf):
    Bsz, H, T, Dk = q.shape
    Dv = v.shape[-1]
    Lc = HGRN_CHUNK
    nc = T // Lc

    def chunks(t):
        return jnp.moveaxis(t.reshape(Bsz, H, nc, Lc, t.shape[-1]), 2, 0)

    causal = jnp.tril(jnp.ones((Lc, Lc), bool))[:, :, None]

    def step(S, inp):
        qc, kc, vc, gc = inp
        b = jnp.cumsum(gc, axis=2)
        decay = jnp.exp(jnp.where(causal, b[:, :, :, None, :] - b[:, :, None, :, :], -jnp.inf))
        attn = jnp.einsum('bhtc,bhsc,bhtsc->bhts', qc, kc, decay)
        o = jnp.einsum('bhtc,bhcv->bhtv', qc * jnp.exp(b), S) + jnp.einsum('bhts,bhsv->bhtv', attn, vc)
        b_end = b[:, :, -1]
        S = jnp.exp(b_end)[..., None] * S + jnp.einsum('bhsc,bhsv->bhcv', kc * jnp.exp(b_end[:, :, None] - b), vc)
        return S, o

    _, o = lax.scan(step, jnp.zeros((Bsz, H, Dk, Dv), jnp.float32),
                    (chunks(q), chunks(k), chunks(v), chunks(logf)))
    return jnp.moveaxis(o, 0, 2).reshape(Bsz, H, T, Dv)


def hgrn2_branch(z, lower_bound, norm_g):
    Bsz, T, _ = z.shape
    f32 = jnp.float32
    q, f, i, g = split_cols(z, [HGRN_WIDTH] * 4)
    fgate = lower_bound + (1.0 - lower_bound) * jax.nn.sigmoid(f.astype(f32))

    def heads(t):
        return t.astype(f32).reshape(Bsz, T, HGRN_HEADS, HEAD_DIM).transpose(0, 2, 1, 3)

    o = hgrn2_chunkwise(heads(jax.nn.silu(q)), heads(1.0 - fgate), heads(i), heads(jnp.log(fgate)))
    o = o.transpose(0, 2, 1, 3)
    o = o * lax.rsqrt(jnp.mean(o * o, axis=-1, keepdims=True) + NORM_EPS)
    return o.reshape(Bsz, T, HGRN_WIDTH) * norm_g * jax.nn.sigmoid(g.astype(f32))


def hybrid_mixer(xn, w_in, b_gate, rwkv_mu, rwkv_w0, rwkv_w2, rwkv_a0, rwkv_a2, rwkv_g2,
                 rwkv_k_k, rwkv_k_a, rwkv_r_k, rwkv_ln_g, rwkv_ln_b, mlstm_conv_w, mlstm_conv_b,
                 mlstm_i_b, mlstm_f_b, lower_bound, hgrn_norm_g, p_rwkv, p_attn, p_mlstm, p_hgrn, w_out):
    Bsz, T, D = xn.shape
    dt = xn.dtype
    z = xn @ w_in
    zA, zB, zC, zD, zG = split_cols(z, [RWKV_IN, ATTN_IN, MLSTM_IN, HGRN_IN, GATE_IN])
    yA = rwkv7_branch(zA, rwkv_mu, rwkv_w0, rwkv_w2, rwkv_a0, rwkv_a2, rwkv_g2,
                      rwkv_k_k, rwkv_k_a, rwkv_r_k, rwkv_ln_g, rwkv_ln_b)
    yB = dilated_attention_branch(zB)
    yC = mlstm_branch(zC, mlstm_conv_w, mlstm_conv_b, mlstm_i_b, mlstm_f_b)
    yD = hgrn2_branch(zD, lower_bound, hgrn_norm_g)
    gates = jax.nn.sigmoid(zG.reshape(Bsz, T, N_BRANCHES, D) + b_gate)
    merged = (gates[:, :, 0] * (yA.astype(dt) @ p_rwkv)
              + gates[:, :, 1] * (yB.astype(dt) @ p_attn)
              + gates[:, :, 2] * (yC.astype(dt) @ p_mlstm)
              + gates[:, :, 3] * (yD.astype(dt) @ p_hgrn))
    return merged @ w_out


def conv_ffn(xn, w_up, conv_w, conv_b, w_down):
    h = causal_dwconv(xn @ w_up, conv_w, conv_b)
    u, gt = split_cols(h, [D_FF, D_FF])
    return (jax.nn.silu(gt) * u) @ w_down


def setup_inputs(seed: int = 0) -> dict:
    key = jax.random.key(seed)
    keys = iter(jax.random.split(key, 40))

    def nrm(shape, scale):
        return scale * jax.random.normal(next(keys), shape, jnp.float32)

    L = DEPTH
    return {
        'x': nrm((BATCH, SEQ, D_MODEL), 1.0),
        'norm_mix_g': 1.0 + nrm((L, D_MODEL), 0.02),
        'w_in': nrm((L, D_MODEL, N_IN), D_MODEL ** -0.5),
        'b_gate': nrm((L, N_BRANCHES, D_MODEL), 0.01),
        'rwkv_mu': jax.random.uniform(next(keys), (L, RWKV_IN), jnp.float32, 0.1, 0.9),
        'rwkv_w0': -1.0 + nrm((L, RWKV_WIDTH), 0.5),
        'rwkv_w2': nrm((L, RWKV_DECAY_RANK, RWKV_WIDTH), 0.1),
        'rwkv_a0': nrm((L, RWKV_WIDTH), 0.1),
        'rwkv_a2': nrm((L, RWKV_AAA_RANK, RWKV_WIDTH), 0.1),
        'rwkv_g2': nrm((L, RWKV_GATE_RANK, RWKV_WIDTH), RWKV_GATE_RANK ** -0.5),
        'rwkv_k_k': 0.85 + nrm((L, RWKV_WIDTH), 0.05),
        'rwkv_k_a': 1.0 + nrm((L, RWKV_WIDTH), 0.05),
        'rwkv_r_k': nrm((L, RWKV_HEADS, HEAD_DIM), 0.1),
        'rwkv_ln_g': 1.0 + nrm((L, RWKV_WIDTH), 0.02),
        'rwkv_ln_b': nrm((L, RWKV_WIDTH), 0.01),
        'mlstm_conv_w': nrm((L, MLSTM_CONV, 2 * MLSTM_WIDTH), MLSTM_CONV ** -0.5),
        'mlstm_conv_b': nrm((L, 2 * MLSTM_WIDTH), 0.01),
        'mlstm_i_b': nrm((L, MLSTM_HEADS), 0.1),
        'mlstm_f_b': 3.0 + nrm((L, MLSTM_HEADS), 0.5),
        'hgrn_lb_logits': nrm((L, HGRN_WIDTH), 0.5),
        'hgrn_norm_g': 1.0 + nrm((L, HGRN_WIDTH), 0.02),
        'p_rwkv': nrm((L, RWKV_WIDTH, D_MODEL), RWKV_WIDTH ** -0.5),
        'p_attn': nrm((L, ATTN_OUT_WIDTH, D_MODEL), ATTN_OUT_WIDTH ** -0.5),
        'p_mlstm': nrm((L, MLSTM_WIDTH, D_MODEL), MLSTM_WIDTH ** -0.5),
        'p_hgrn': nrm((L, HGRN_WIDTH, D_MODEL), HGRN_WIDTH ** -0.5),
        'w_out': nrm((L, D_MODEL, D_MODEL), D_MODEL ** -0.5),
        'norm_ffn_g': 1.0 + nrm((L, D_MODEL), 0.02),
        'w_up': nrm((L, D_MODEL, 2 * D_FF), D_MODEL ** -0.5),
        'ffn_conv_w': nrm((L, FFN_CONV, 2 * D_FF), FFN_CONV ** -0.5),
        'ffn_conv_b': nrm((L, 2 * D_FF), 0.01),
        'w_down': nrm((L, D_FF, D_MODEL), D_FF ** -0.5),
        'final_norm_g': 1.0 + nrm((D_MODEL,), 0.02),
    }


def reference(x, norm_mix_g, w_in, b_gate, rwkv_mu, rwkv_w0, rwkv_w2, rwkv_a0, rwkv_a2, rwkv_g2,
              rwkv_k_k, rwkv_k_a, rwkv_r_k, rwkv_ln_g, rwkv_ln_b, mlstm_conv_w, mlstm_conv_b,
              mlstm_i_b, mlstm_f_b, hgrn_lb_logits, hgrn_norm_g, p_rwkv, p_attn, p_mlstm, p_hgrn,
              w_out, norm_ffn_g, w_up, ffn_conv_w, ffn_conv_b, w_down, final_norm_g):
    lb_p = jax.nn.softmax(hgrn_lb_logits.astype(jnp.float32), axis=0)
    lower_bounds = jnp.cumsum(lb_p, axis=0) - lb_p[0]
    h = x
    for l in range(DEPTH):
        xn = rmsnorm(h, norm_mix_g[l])
        mix = hybrid_mixer(xn, w_in[l], b_gate[l], rwkv_mu[l], rwkv_w0[l], rwkv_w2[l], rwkv_a0[l],
                           rwkv_a2[l], rwkv_g2[l], rwkv_k_k[l], rwkv_k_a[l], rwkv_r_k[l], rwkv_ln_g[l],
                           rwkv_ln_b[l], mlstm_conv_w[l], mlstm_conv_b[l], mlstm_i_b[l], mlstm_f_b[l],
                           lower_bounds[l], hgrn_norm_g[l], p_rwkv[l], p_attn[l], p_mlstm[l], p_hgrn[l],
                           w_out[l])
        h = h + mix.astype(h.dtype)
        xn = rmsnorm(h, norm_ffn_g[l])
        h = h + conv_ffn(xn, w_up[l], ffn_conv_w[l], ffn_conv_b[l], w_down[l]).astype(h.dtype)
    return rmsnorm(h, final_norm_g)
```

```python
import numpy as np
from contextlib import ExitStack
import concourse.bass as bass
import concourse.mybir as mybir
from concourse.bass_utils import run_bass_kernel_spmd

F32 = mybir.dt.float32
BF16 = mybir.dt.bfloat16
AF = mybir.ActivationFunctionType
ALU = mybir.AluOpType

T = 2048
D = 1024
DEPTH = 2
NTT = 16
DFF = 2816
NFT = 22
N_IN = 8200
EPS = 1e-6


class Buf:
    __slots__ = ("w", "r", "name", "psum")

    def __init__(self, name="", psum=False):
        self.w = None
        self.r = []
        self.name = name
        self.psum = psum


class TT:
    def __init__(self, t, b):
        self.t = t
        self.b = b

    def __getitem__(self, k):
        return self.t[k]


class Prog:
    ENG = ("pe", "act", "dve", "pool", "sp")
    NDMA = 8

    def __init__(self, nc, es):
        self.nc = nc
        self.es = es
        self.ops = {e: [] for e in self.ENG}
        self.count = {e: 0 for e in self.ENG}
        self.epoch = {e: 0 for e in self.ENG}
        self.sem = {e: es.enter_context(nc.semaphore("sem_" + e)) for e in self.ENG if e != "sp"}
        self.known = {e: {} for e in self.ENG}
        self.dsem = {}
        self.dcount = {}
        self.dnext = {}
        for q in ("sp", "act", "pool"):
            self.dsem[q] = [es.enter_context(nc.semaphore(f"dsem_{q}{i}")) for i in range(self.NDMA)]
            self.dcount[q] = [0] * self.NDMA
            self.dnext[q] = 0
        self.semobj = dict(self.sem)
        for q in self.dsem:
            for i, s in enumerate(self.dsem[q]):
                self.semobj[("d", q, i)] = s
        self.n_wait = 0
        self.n_ops = 0

    def sb(self, name, shape, dt=F32):
        t = self.es.enter_context(self.nc.sbuf_tensor(name, list(shape), dt))
        return TT(t, Buf(name))

    def ps(self, name, shape, dt=F32):
        t = self.es.enter_context(self.nc.psum_tensor(name, list(shape), dt))
        return TT(t, Buf(name, psum=True))

    @staticmethod
    def _bl(xs):
        out = []
        for x in xs:
            if isinstance(x, TT):
                if isinstance(x.b, list):
                    out.extend(x.b)
                else:
                    out.append(x.b)
            elif isinstance(x, (list, tuple)):
                out.extend(Prog._bl(x))
            else:
                out.append(x)
        return out

    def _deps(self, reads, writes):
        deps = {}

        def add(tok):
            k, v = tok
            if v > deps.get(k, 0):
                deps[k] = v

        for b in reads:
            if b.w is not None:
                add(b.w)
            if b.psum:
                for r in b.r:
                    add(r)
        for b in writes:
            if b.w is not None:
                add(b.w)
            for r in b.r:
                add(r)
        return deps

    def _commit(self, reads, writes, tok):
        for b in reads:
            b.r.append(tok)
            if len(b.r) > 64:
                mx = {}
                for k, v in b.r:
                    if v > mx.get(k, 0):
                        mx[k] = v
                b.r = list(mx.items())
        for b in writes:
            b.w = tok
            b.r = []

    EPOCH = 8000

    def _ekey(self, e):
        ep = self.epoch[e]
        return e if ep == 0 else f"{e}#{ep}"

    def op(self, e, fn, reads=(), writes=()):
        reads = self._bl(reads)
        writes = self._bl(writes)
        if self.count[e] >= self.EPOCH:
            self.epoch[e] += 1
            self.count[e] = 0
            nk = self._ekey(e)
            self.semobj[nk] = self.es.enter_context(self.nc.semaphore("sem_" + nk.replace("#", "_")))
        ek = self._ekey(e)
        idx = self.count[e] + 1
        deps = self._deps(reads, writes)
        waits = []
        kn = self.known[e]
        for k, v in deps.items():
            kb = k.split("#")[0] if isinstance(k, str) else None
            if kb == e:
                if e == "pe":
                    continue
                if k == ek and v <= idx - 6:
                    continue
            if v > kn.get(k, 0):
                waits.append((k, v))
                kn[k] = v
        self.count[e] = idx
        tok = (ek, idx)
        self.ops[e].append((waits, fn, (ek, 1)))
        self.n_wait += len(waits)
        self.n_ops += 1
        self._commit(reads, writes, tok)
        return tok

    def dma(self, q, out, in_, reads=(), writes=()):
        reads = self._bl(reads)
        writes = self._bl(writes)
        j = self.dnext[q]
        self.dnext[q] = (j + 1) % self.NDMA
        key = ("d", q, j)
        prev = self.dcount[q][j]
        deps = self._deps(reads, writes)
        if prev > 0:
            deps[key] = max(deps.get(key, 0), 16 * prev)
        waits = []
        kn = self.known[q]
        for k, v in deps.items():
            if v > kn.get(k, 0):
                waits.append((k, v))
                kn[k] = v
        self.dcount[q][j] = prev + 1
        tok = (key, 16 * (prev + 1))

        def fn(eng):
            return eng.dma_start(out=out, in_=in_)

        self.ops[q].append((waits, fn, (key, 16)))
        self.n_wait += len(waits)
        self.n_ops += 1
        self._commit(reads, writes, tok)
        return tok

    def finish_wait(self, e, toks):
        self.ops[e].append((list(toks), None, None))

    def emit(self):
        nc = self.nc
        with nc.Block() as block:
            def run(ename, eng):
                for waits, fn, inc in self.ops[ename]:
                    for k, v in waits:
                        eng.wait_ge(self.semobj[k], v)
                    if fn is not None:
                        ins = fn(eng)
                        ins.then_inc(self.semobj[inc[0]], inc[1])

            @block.sync
            def _(eng):
                run("sp", eng)

            @block.scalar
            def _(eng):
                run("act", eng)

            @block.vector
            def _(eng):
                run("dve", eng)

            @block.gpsimd
            def _(eng):
                run("pool", eng)

            @block.tensor
            def _(eng):
                run("pe", eng)


OA, OB, OC, OD, OG = 0, 896, 2048, 3080, 4104


def _win_cols():
    tiles = {}
    cols = []

    def add(name, idx):
        idx = np.asarray(idx, dtype=np.int64)
        assert idx.size == 128
        tiles[name] = len(cols)
        cols.append(idx)

    ar = np.arange
    for i in range(2):
        add(f"A_r{i}", OA + i * 128 + ar(128))
        add(f"A_k{i}", OA + 256 + i * 128 + ar(128))
        add(f"A_v{i}", OA + 512 + i * 128 + ar(128))
    add("A_m", OA + 768 + ar(128))
    perm = np.concatenate([ar(32) + 32, ar(32)])
    perm128 = np.concatenate([perm, perm + 64])
    for g in range(3):
        add(f"B_q{g}", OB + g * 128 + ar(128))
        add(f"B_k{g}", OB + 384 + g * 128 + ar(128))
        add(f"B_v{g}", OB + 768 + g * 128 + ar(128))
    for i in range(2):
        add(f"C_q{i}", OC + i * 128 + ar(128))
        add(f"C_k{i}", OC + 256 + i * 128 + ar(128))
        add(f"C_v{i}", OC + 512 + i * 128 + ar(128))
        add(f"C_o{i}", OC + 768 + i * 128 + ar(128))
        add(f"C_ig{i}", OC + 1024 + np.repeat(ar(2) + 2 * i, 64))
        add(f"C_fg{i}", OC + 1028 + np.repeat(ar(2) + 2 * i, 64))
    for i in range(2):
        add(f"D_q{i}", OD + i * 128 + ar(128))
        add(f"D_f{i}", OD + 256 + i * 128 + ar(128))
        add(f"D_i{i}", OD + 512 + i * 128 + ar(128))
        add(f"D_g{i}", OD + 768 + i * 128 + ar(128))
    for b in range(4):
        for dt in range(8):
            add(f"G_{b}_{dt}", OG + b * 1024 + dt * 128 + ar(128))
    return tiles, np.concatenate(cols)


WIN_TILES, WIN_COLS = _win_cols()
NWT = len(WIN_TILES)

VEC_SPEC = [
    ("nmg", 1024), ("nfg", 1024), ("bgate", 4096),
    ("mu_r", 256), ("mu_k", 256), ("mu_v", 256), ("mu_m", 128),
    ("w0", 256), ("a0", 256), ("k_k", 256), ("k_a", 256), ("r_k", 256), ("ln_g", 256), ("ln_b", 256),
    ("mcw0", 512), ("mcw1", 512), ("mcw2", 512), ("mcw3", 512), ("mcb", 512),
    ("i_b", 256), ("f_b", 256), ("lb0", 256), ("lb1", 256), ("hng", 256),
    ("fcw0", 5632), ("fcw1", 5632), ("fcw2", 5632), ("fcb", 5632), ("fing", 1024),
]
VEC_OFF = {}
_o = 0
for _n, _l in VEC_SPEC:
    VEC_OFF[_n] = _o
    _o += _l // 128
NVEC = _o


def _pack_vecs(inp, l):
    v = {
        "nmg": inp["norm_mix_g"][l], "nfg": inp["norm_ffn_g"][l], "bgate": inp["b_gate"][l].reshape(-1),
        "mu_r": inp["rwkv_mu"][l][0:256], "mu_k": inp["rwkv_mu"][l][256:512], "mu_v": inp["rwkv_mu"][l][512:768],
        "mu_m": inp["rwkv_mu"][l][768:896],
        "w0": inp["rwkv_w0"][l], "a0": inp["rwkv_a0"][l], "k_k": inp["rwkv_k_k"][l], "k_a": inp["rwkv_k_a"][l],
        "r_k": inp["rwkv_r_k"][l].reshape(-1), "ln_g": inp["rwkv_ln_g"][l], "ln_b": inp["rwkv_ln_b"][l],
        "mcw0": inp["mlstm_conv_w"][l][0], "mcw1": inp["mlstm_conv_w"][l][1], "mcw2": inp["mlstm_conv_w"][l][2],
        "mcw3": inp["mlstm_conv_w"][l][3], "mcb": inp["mlstm_conv_b"][l],
        "i_b": np.repeat(inp["mlstm_i_b"][l], 64), "f_b": np.repeat(inp["mlstm_f_b"][l], 64),
        "lb0": inp["hgrn_lb_logits"][0], "lb1": inp["hgrn_lb_logits"][1], "hng": inp["hgrn_norm_g"][l],
        "fcw0": inp["ffn_conv_w"][l][0], "fcw1": inp["ffn_conv_w"][l][1], "fcw2": inp["ffn_conv_w"][l][2],
        "fcb": inp["ffn_conv_b"][l], "fing": inp["final_norm_g"],
    }
    out = np.zeros((128, NVEC), np.float32)
    for n, ln in VEC_SPEC:
        a = np.asarray(v[n], np.float32).reshape(ln // 128, 128)
        out[:, VEC_OFF[n]:VEC_OFF[n] + ln // 128] = a.T
    return out


CST_SPEC = [("ident", 128), ("ones", 128), ("bd64", 128), ("rot", 128), ("m_gla", 256), ("m_rw", 512), ("m_rwn", 128), ("m_rwn4", 512),
            ("m_att", 512), ("opad", 256), ("cos", 2048), ("sin", 2048)]
CST_OFF = {}
_o = 0
for _n, _l in CST_SPEC:
    CST_OFF[_n] = _o
    _o += _l
NCST = _o


def _consts():
    c = np.zeros((128, NCST), np.float32)
    p = np.arange(128)[:, None]
    f = np.arange(128)[None, :]
    c[:, CST_OFF["ident"]:CST_OFF["ident"] + 128] = (p == f)
    c[:, CST_OFF["ones"]:CST_OFF["ones"] + 128] = 1.0
    c[:, CST_OFF["bd64"]:CST_OFF["bd64"] + 128] = (p // 64 == f // 64)
    rot = np.where((f % 64 < 32) & (p == f + 32), -1.0, 0.0) + np.where((f % 64 >= 32) & (p == f - 32), 1.0, 0.0)
    c[:, CST_OFF["rot"]:CST_OFF["rot"] + 128] = rot
    m = ((p // 64 == f // 64) & (p <= f)).astype(np.float32)
    c[:, CST_OFF["m_gla"]:CST_OFF["m_gla"] + 256] = np.tile(m, (1, 2))
    strict = (p < f).astype(np.float32)
    incl = (p <= f).astype(np.float32)
    c[:, CST_OFF["m_rw"]:CST_OFF["m_rw"] + 512] = np.concatenate([strict, incl, strict, incl], axis=1)
    c[:, CST_OFF["m_rwn"]:CST_OFF["m_rwn"] + 128] = (f < p)
    c[:, CST_OFF["m_rwn4"]:CST_OFF["m_rwn4"] + 512] = np.tile((f < p).astype(np.float32), (1, 4))
    cur = (p <= f).astype(np.float32)
    prv = (p >= f).astype(np.float32)
    c[:, CST_OFF["m_att"]:CST_OFF["m_att"] + 512] = np.concatenate([cur, prv, cur, prv], axis=1)
    op = np.zeros((128, 256), np.float32)
    op[:, 0:64] = 1.0
    op[:, 128 + 64:256] = 1.0
    c[:, CST_OFF["opad"]:CST_OFF["opad"] + 256] = op
    half = 32
    inv = 10000.0 ** (-np.arange(half, dtype=np.float32) / half)
    cidx = np.arange(128) % 64
    ang = np.arange(T, dtype=np.float32)[None, :] * inv[cidx % 32][:, None]
    c[:, CST_OFF["cos"]:CST_OFF["cos"] + T] = np.cos(ang)
    c[:, CST_OFF["sin"]:CST_OFF["sin"] + T] = np.sin(ang)
    return c


class Kern:
    def __init__(self, nc, P, dr, dbg=None):
        self.nc = nc
        self.P = P
        self.dr = dr
        self.dbg = dbg
        self.dbg_toks = []
        self.psi = 0
        self.nrot = 6
        self.wbi = 0
        sb = P.sb
        self.PS = [P.ps(f"ps{i}", [128, 512], F32) for i in range(8)]
        self.FS = [sb(f"fs{i}", [128, T], F32) for i in range(9)]
        self.HS = [sb(f"hs{i}", [128, T], BF16) for i in range(9)]
        self.xnT = sb("xnT", [128, 8, T], BF16)
        self.WB = [sb(f"wb{i}", [128, 1024], BF16) for i in range(4)]
        yab = sb("yAB", [128, 3 * T], BF16)
        yc = sb("yCt", [128, 2 * T], BF16)
        yd = sb("yDt", [128, 2 * T], BF16)
        self.yA = [TT(yab.t[:, i * T:(i + 1) * T], Buf(f"yA{i}")) for i in range(2)]
        self.yB = [TT(yab.t[:, 2 * T:3 * T], Buf("yB0"))]
        self.yC = [TT(yc.t[:, i * T:(i + 1) * T], Buf(f"yC{i}")) for i in range(2)]
        self.yD = [TT(yd.t[:, i * T:(i + 1) * T], Buf(f"yD{i}")) for i in range(2)]
        self.cg = TT(yab.t[:, 0:2 * T].bitcast(F32), [self.yA[0].b, self.yA[1].b])
        self.AR = TT(yc.t[:, :], [self.yC[0].b, self.yC[1].b])
        self.ydt = yd
        self.xu = [TT(yd.t[:, 3584 + j * 128:3584 + (j + 1) * 128], Buf(f"xu{j}")) for j in range(4)]
        self.nm_bufs = []
        self.hb = [Buf(f"hscr{i}") for i in range(8)]
        self.vec = [sb(f"vec{l}", [128, NVEC], F32) for l in range(DEPTH)]
        self.identF = sb("identF", [128, 128], F32)
        self.cst = {}
        for n, ln in CST_SPEC:
            if n in ("cos", "sin"):
                continue
            self.cst[n] = sb("c_" + n, [128, ln], BF16)
        self.small = sb("small", [128, 64], F32)
        self.tmpA = [sb(f"tmpA{i}", [128, 512], F32) for i in range(3)]
        self.tmpB = [sb(f"tmpB{i}", [128, 512], BF16) for i in range(3)]
        self.tai = 0
        self.tbi = 0
        self.Sb = TT(self.FS[8].t[:, :].bitcast(BF16).rearrange("p (n v) -> p n v", v=128), self.FS[8].b)
        self.pcx = sb("pcx", [128, 3 + T], F32)
        self.Sf = [sb(f"Sf{i}", [128, 128], F32) for i in range(2)]
        self.chs = sb("chs", [128, 4, 32], F32)
        self.misc = sb("miscw_sb", [128, 256], BF16)

    def nps(self):
        p = self.PS[self.psi]
        self.psi = (self.psi + 1) % self.nrot
        return p

    def ta(self):
        t = self.tmpA[self.tai]
        self.tai = (self.tai + 1) % 3
        return t

    def tb(self):
        t = self.tmpB[self.tbi]
        self.tbi = (self.tbi + 1) % 3
        return t

    def mm(self, out, lhsT, rhs, start, stop, reads, writes):
        self.P.op("pe", lambda e: e.matmul(out, lhsT=lhsT, rhs=rhs, start=start, stop=stop), reads, writes)

    def tr(self, out, in_, ident, reads, writes):
        self.P.op("pe", lambda e: e.transpose(out, in_, ident), reads, writes)

    def act(self, out, in_, func, reads, writes, scale=None, bias=None):
        kw = {}
        if scale is not None:
            kw["scale"] = scale
        if bias is not None:
            kw["bias"] = bias
        self.P.op("act", lambda e: e.activation(out=out, in_=in_, func=func, **kw), reads, writes)

    def tt(self, out, in0, in1, op, reads, writes, eng="dve"):
        self.P.op(eng, lambda e: e.tensor_tensor(out=out, in0=in0, in1=in1, op=op), reads, writes)

    def ts(self, out, in0, s1, s2, op0, op1, reads, writes, eng="dve"):
        if op1 is None:
            self.P.op(eng, lambda e: e.tensor_scalar(out=out, in0=in0, scalar1=s1, scalar2=None, op0=op0), reads, writes)
        else:
            self.P.op(eng, lambda e: e.tensor_scalar(out=out, in0=in0, scalar1=s1, scalar2=s2, op0=op0, op1=op1), reads, writes)

    def stt(self, out, in0, scalar, in1, op0, op1, reads, writes):
        self.P.op("dve", lambda e: e.scalar_tensor_tensor(out=out, in0=in0, scalar=scalar, in1=in1, op0=op0, op1=op1), reads, writes)

    def cp(self, out, in_, reads, writes, eng="act"):
        if eng == "act":
            self.P.op("act", lambda e: e.copy(out=out, in_=in_), reads, writes)
        else:
            self.P.op(eng, lambda e: e.tensor_copy(out=out, in_=in_), reads, writes)

    def memset(self, ap, val, writes, eng="dve"):
        self.P.op(eng, lambda e: e.memset(ap, val), (), writes)

    def vcol(self, l, name, j=0):
        o = VEC_OFF[name] + j
        return self.vec[l][:, o:o + 1]

    def dump(self, idx, tt_, ap):
        if self.dbg is None:
            return
        tok = self.P.dma("pool", self.dbg[idx], ap, reads=[tt_])
        self.dbg_toks.append(tok)

    def wload(self, src_ap, kc, m):
        wb = self.WB[self.wbi]
        self.wbi = (self.wbi + 1) % len(self.WB)
        view = wb.t[:, 0:kc * m].rearrange("p (k m) -> p k m", m=m)
        self.P.dma("pool", view, src_ap, writes=[wb])
        return wb, view

    def win_tile(self, l, name):
        c = WIN_TILES[name]
        src = self.dr["w_in"][l, :, c * 128:(c + 1) * 128].rearrange("(k p) m -> p k m", p=128)
        return self.wload(src, 8, 128)

    def projT(self, wb, wv, consume, rhs=None):
        for tb in range(4):
            ps = self.nps()
            for kc in range(8):
                self.mm(ps[:, :], wv[:, kc, :], self.xnT[:, kc, tb * 512:(tb + 1) * 512], kc == 0, kc == 7,
                        [wb, self.xnT], [ps])
            consume(tb, ps)

    def setup(self):
        P = self.P
        dr = self.dr
        P.dma("sp", self.identF[:, :], dr["cst"][:, CST_OFF["ident"]:CST_OFF["ident"] + 128], writes=[self.identF])
        for n, ln in CST_SPEC:
            if n in ("cos", "sin"):
                continue
            P.dma("pool", self.cst[n][:, :], dr["cst"][:, CST_OFF[n]:CST_OFF[n] + ln], writes=[self.cst[n]])
        for l in range(DEPTH):
            P.dma("sp", self.vec[l][:, :], dr["vecs"][l], writes=[self.vec[l]])
        self.memset(self.small[:, :], 0.0, [self.small])
        self.memset(self.small[:, 0:1], 1.0, [self.small])

    def load_x(self):
        P = self.P
        xin = [self.FS[8], None]
        for tt_ in range(NTT):
            half = tt_ % 2
            stg = self.FS[8].t[:, half * 1024:(half + 1) * 1024]
            if tt_ < 2:
                if tt_ == 0:
                    self._xb = [Buf("xin0"), Buf("xin1")]
            xb = self._xb[half]
            P.dma("sp", stg, self.dr["x"][tt_ * 128:(tt_ + 1) * 128, :], writes=[xb])
            for kc in range(8):
                ps = self.nps()
                self.tr(ps[:, 0:128], stg[:, kc * 128:(kc + 1) * 128], self.identF[:, :], [xb, self.identF], [ps])
                if kc % 2 == 0:
                    self.cp(self.FS[kc][:, tt_ * 128:(tt_ + 1) * 128], ps[:, 0:128], [ps], [self.FS[kc]])
                else:
                    self.cp(self.FS[kc][:, tt_ * 128:(tt_ + 1) * 128], ps[:, 0:128], [ps], [self.FS[kc]], eng="dve")
        self.FS[8].b.r.extend(self._xb[0].r + self._xb[1].r)
        if self._xb[0].w:
            self.FS[8].b.r.append(self._xb[0].w)
        if self._xb[1].w:
            self.FS[8].b.r.append(self._xb[1].w)

    def rmsnorm(self, l, gname, to_xn=True, out_fn=None):
        ones = self.cst["ones"]
        for tb in range(4):
            sl = slice(tb * 512, (tb + 1) * 512)
            ps = self.nps()
            for kc in range(8):
                sq = self.tb()
                self.act(sq[:, :], self.FS[kc][:, sl], AF.Square, [self.FS[kc]], [sq])
                self.mm(ps[:, :], ones[:, :], sq[:, :], kc == 0, kc == 7, [ones, sq], [ps])
            lnv = self.ta()
            self.act(lnv[:, :], ps[:, :], AF.Ln, [ps], [lnv], scale=1.0 / D, bias=self.small[:, 1:2])
            rstd = self.ta()
            self.act(rstd[:, :], lnv[:, :], AF.Exp, [lnv], [rstd], scale=-0.5)
            for kc in range(8):
                if to_xn:
                    self.stt(self.xnT[:, kc, sl], self.FS[kc][:, sl], self.vcol(l, gname, kc), rstd[:, :],
                             ALU.mult, ALU.mult, [self.FS[kc], rstd, self.vec[l]], [self.xnT])
                else:
                    out_fn(tb, kc, rstd)

    def gla(self, qF, kF, GF, Dt, Et, Hq, Hk, Hqh, Hkh, khtm, vsel, vrows, vdeps, DV, out_fn, DE2):
        P = self.P
        chs = self.chs
        onecol = self.small[:, 0:1]
        P.op("dve", lambda e: e.tensor_tensor_scan(out=GF[:, :], data0=onecol.to_broadcast([128, T]), data1=GF[:, :],
                                                   initial=0.0, op0=ALU.mult, op1=ALU.add), [GF, self.small], [GF])
        B3 = GF[:, :].rearrange("p (n l) -> p n l", l=64)
        D3 = Dt[:, :].rearrange("p (n l) -> p n l", l=64)
        self.memset(chs[:, 0, 0:1], 0.0, [chs])
        self.cp(chs[:, 0, 1:32], GF[:, 63:T - 64:64], [GF], [chs], eng="dve")
        self.cp(chs[:, 1, :], GF[:, 63:T:64], [GF], [chs], eng="dve")
        self.tt(chs[:, 3, :], chs[:, 1, :], chs[:, 0, :], ALU.subtract, [chs], [chs])
        self.act(chs[:, 2, :], chs[:, 3, :], AF.Exp, [chs], [chs])
        bmid = B3[:, :, 31:32].to_broadcast([128, 32, 64])
        bp = chs[:, 0, :].unsqueeze(2).to_broadcast([128, 32, 64])
        be = chs[:, 1, :].unsqueeze(2).to_broadcast([128, 32, 64])
        Dt2, Et2 = DE2
        D3b = Dt2[:, :].rearrange("p (n l) -> p n l", l=64)
        self.tt(D3, B3, bmid, ALU.subtract, [GF], [Dt])
        self.act(Et[:, :], Dt[:, :], AF.Exp, [Dt], [Et])
        self.act(Et2[:, :], Dt[:, :], AF.Exp, [Dt], [Et2], scale=-1.0)
        self.tt(D3b, B3, bp, ALU.subtract, [GF, chs], [Dt2])
        self.tt(Hq[:, :], qF[:, :], Et[:, :], ALU.mult, [qF, Et], [Hq])
        self.tt(Hk[:, :], kF[:, :], Et2[:, :], ALU.mult, [kF, Et2], [Hk])
        self.act(Et[:, :], Dt2[:, :], AF.Exp, [Dt2], [Et])
        self.tt(D3, be, B3, ALU.subtract, [GF, chs], [Dt])
        self.act(Et2[:, :], Dt[:, :], AF.Exp, [Dt], [Et2])
        self.tt(Hqh[:, :], qF[:, :], Et[:, :], ALU.mult, [qF, Et], [Hqh])
        self.tt(Hkh[:, :], kF[:, :], Et2[:, :], ALU.mult, [kF, Et2], [Hkh])
        self.to_tm(Hkh, khtm)
        kh3 = khtm[:, :].rearrange("p (t c) -> p t c", c=128)
        Sb = self.Sb
        self.memset(Sb[:, 0, :], 0.0, [Sb])
        self.memset(self.Sf[0][:, :], 0.0, [self.Sf[0]])
        per = 512 // DV
        psh = [None, None]
        for n in range(32):
            tt_, half = n // 2, n % 2
            if tt_ % per == 0:
                psh[half] = self.nps()
            ps = psh[half]
            c0 = (tt_ % per) * DV
            for hh in range(2):
                po = hh * 64
                self.mm(ps[po:po + 64, c0:c0 + DV], kh3[half * 64:half * 64 + 64, tt_, po:po + 64], vrows(tt_, half, hh),
                        True, True, [khtm] + vdeps, [ps])
            sfo, sfn = self.Sf[n % 2], self.Sf[(n + 1) % 2]
            self.stt(sfn[:, 0:DV], sfo[:, 0:DV], chs[:, 2, n:n + 1], ps[:, c0:c0 + DV], ALU.mult, ALU.add,
                     [sfo, chs, ps], [sfn])
            if n < 31:
                self.cp(Sb[:, n + 1, 0:DV], sfn[:, 0:DV], [sfn], [Sb])
        mg = self.cst["m_gla"]
        pso = [None, None]

        def scores(tt_):
            cs = slice(tt_ * 128, (tt_ + 1) * 128)
            at = self.tb()
            for hh in range(2):
                po = hh * 64
                pss = self.nps()
                self.mm(pss[:, 0:128], Hk[po:po + 64, cs], Hq[po:po + 64, cs], True, True, [Hk, Hq], [pss])
                self.tt(at[:, hh * 128:(hh + 1) * 128], pss[:, 0:128], mg[:, 0:128], ALU.mult, [pss, mg], [at])
            return at

        ats = {0: scores(0)}
        for tt_ in range(NTT):
            if tt_ + 1 < NTT:
                ats[tt_ + 1] = scores(tt_ + 1)
            at = ats.pop(tt_)
            if tt_ % 4 == 0:
                pso = [self.PS[6], self.PS[7]]
            c0 = (tt_ % 4) * 128
            for hh in range(2):
                po = hh * 64
                po_ = pso[hh]
                self.mm(po_[0:DV, c0:c0 + 128], vsel(tt_, hh), at[:, hh * 128:(hh + 1) * 128], True, False,
                        vdeps + [at], [po_])
                for half in range(2):
                    n = 2 * tt_ + half
                    self.mm(po_[0:DV, c0 + half * 64:c0 + half * 64 + 64], Sb[po:po + 64, n, 0:DV],
                            Hqh[po:po + 64, n * 64:(n + 1) * 64], False, half == 1, [Sb, Hqh], [po_])
            if tt_ % 4 == 3:
                for hh in range(2):
                    out_fn(hh, tt_ // 4, pso[hh])

    def to_tm(self, srcH, dst, dview=None):
        idb = self.cst["ident"]
        d3 = dst[:, :].rearrange("p (t c) -> p t c", c=128) if dview is None else dview
        for g4 in range(4):
            ps = self.nps()
            pb = ps.t[:, :].bitcast(BF16)
            for j in range(4):
                tt_ = g4 * 4 + j
                self.tr(pb[:, j * 128:(j + 1) * 128], srcH[:, tt_ * 128:(tt_ + 1) * 128], idb[:, :], [srcH, idb], [ps])
            self.cp(d3[:, g4 * 4:(g4 + 1) * 4, :], pb[:, 0:512].rearrange("p (t c) -> p t c", c=128), [ps], [dst])

    def hgrn2(self, l):
        FS, HS = self.FS, self.HS
        for i in range(2):
            qF, kF, GF, Dt, Et, oT = FS[0], FS[1], FS[2], FS[3], FS[4], FS[5]
            Hq, Hk, Hqh, Hkh, Hv, Hg, khtm, vtm = HS[0], HS[1], HS[2], HS[3], HS[4], HS[5], HS[6], HS[7]
            lbc = self.small[:, 4 + i:5 + i]
            omc = self.small[:, 6 + i:7 + i]
            wb, wv = self.win_tile(l, f"D_q{i}")
            self.projT(wb, wv, lambda tb, ps: self.act(qF[:, tb * 512:(tb + 1) * 512], ps[:, :], AF.Silu, [ps], [qF]))
            wb, wv = self.win_tile(l, f"D_f{i}")
            self.projT(wb, wv, lambda tb, ps: self.act(GF[:, tb * 512:(tb + 1) * 512], ps[:, :], AF.Sigmoid, [ps], [GF]))
            self.ts(GF[:, :], GF[:, :], omc, lbc, ALU.mult, ALU.add, [GF, self.small], [GF])
            self.ts(kF[:, :], GF[:, :], -1.0, 1.0, ALU.mult, ALU.add, [GF], [kF])
            self.act(GF[:, :], GF[:, :], AF.Ln, [GF], [GF])
            wb, wv = self.win_tile(l, f"D_i{i}")
            self.projT(wb, wv, lambda tb, ps: self.cp(Hv[:, tb * 512:(tb + 1) * 512], ps[:, :], [ps], [Hv]))
            wb, wv = self.win_tile(l, f"D_g{i}")
            self.projT(wb, wv, lambda tb, ps: self.act(Hg[:, tb * 512:(tb + 1) * 512], ps[:, :], AF.Sigmoid, [ps], [Hg]))
            self.to_tm(Hv, vtm)
            v3 = vtm[:, :].rearrange("p (t c) -> p t c", c=128)

            def out_fn(hh, g4, ps, oT=oT):
                self.cp(oT[hh * 64:hh * 64 + 64, g4 * 512:(g4 + 1) * 512], ps[0:64, :], [ps], [oT])

            self.gla(qF, kF, GF, Dt, Et, Hq, Hk, Hqh, Hkh, khtm,
                     lambda tt_, hh: v3[:, tt_, hh * 64:hh * 64 + 64],
                     lambda tt_, half, hh: v3[half * 64:half * 64 + 64, tt_, hh * 64:hh * 64 + 64],
                     [vtm], 64, out_fn, (FS[6], FS[7]))
            bd = self.cst["bd64"]
            for tb in range(4):
                sl = slice(tb * 512, (tb + 1) * 512)
                sq = self.tb()
                self.act(sq[:, :], oT[:, sl], AF.Square, [oT], [sq])
                ps = self.nps()
                self.mm(ps[:, :], bd[:, :], sq[:, :], True, True, [bd, sq], [ps])
                lnv = self.ta()
                self.act(lnv[:, :], ps[:, :], AF.Ln, [ps], [lnv], scale=1.0 / 64, bias=self.small[:, 1:2])
                rstd = self.ta()
                self.act(rstd[:, :], lnv[:, :], AF.Exp, [lnv], [rstd], scale=-0.5)
                self.tt(rstd[:, :], rstd[:, :], oT[:, sl], ALU.mult, [rstd, oT], [rstd])
                self.stt(self.yD[i][:, sl], rstd[:, :], self.vcol(l, "hng", i), Hg[:, sl], ALU.mult, ALU.mult,
                         [rstd, Hg, self.vec[l]], [self.yD[i]])

    def conv4(self, l, dst, stg, ci):
        w = lambda j: self.vcol(l, f"mcw{j}", ci)
        self.act(dst[:, :], stg[:, 0:T], AF.Identity, [stg, self.vec[l]], [dst], scale=w(3), bias=self.vcol(l, "mcb", ci))
        for j, sh in ((2, 1), (1, 2), (0, 3)):
            self.stt(dst[:, sh:T], stg[:, 0:T - sh], w(j), dst[:, sh:T], ALU.mult, ALU.add, [stg, dst, self.vec[l]], [dst])
        self.act(dst[:, :], dst[:, :], AF.Silu, [dst], [dst])

    def mlstm(self, l):
        FS, HS = self.FS, self.HS
        for i in range(2):
            qF, kF, GF, Dt, Et, hT, pcs, vaug_s = FS[0], FS[1], FS[2], FS[3], FS[4], FS[5], FS[6], FS[7]
            Hq, Hk, Hqh, Hkh, Hv, Hso, khtm = HS[0], HS[1], HS[2], HS[3], HS[4], HS[5], HS[6]
            stq = TT(self.pcx.t[:, 0:T], self.pcx.b)
            stk = pcs
            wb, wv = self.win_tile(l, f"C_q{i}")
            self.projT(wb, wv, lambda tb, ps: self.cp(stq[:, tb * 512:(tb + 1) * 512], ps[:, :], [ps], [stq]))
            wb2, wv2 = self.win_tile(l, f"C_k{i}")
            self.projT(wb2, wv2, lambda tb, ps: self.cp(stk[:, tb * 512:(tb + 1) * 512], ps[:, :], [ps], [stk]))
            self.conv4(l, qF, stq, i)
            self.conv4(l, kF, stk, 2 + i)
            wb, wv = self.win_tile(l, f"C_ig{i}")
            self.projT(wb, wv, lambda tb, ps: self.act(Et[:, tb * 512:(tb + 1) * 512], ps[:, :], AF.Exp, [ps, self.vec[l]], [Et],
                                                       bias=self.vcol(l, "i_b", i)))
            self.stt(kF[:, :], kF[:, :], 0.125, Et[:, :], ALU.mult, ALU.mult, [kF, Et], [kF])
            wb, wv = self.win_tile(l, f"C_fg{i}")
            nfb = self.small[:, 8 + i:9 + i]
            self.projT(wb, wv, lambda tb, ps: self.act(GF[:, tb * 512:(tb + 1) * 512], ps[:, :], AF.Exp, [ps, self.small], [GF],
                                                       scale=-1.0, bias=nfb))
            self.act(GF[:, :], GF[:, :], AF.Ln, [GF, self.small], [GF], bias=self.small[:, 0:1])
            self.ts(GF[:, :], GF[:, :], -1.0, None, ALU.mult, None, [GF], [GF])
            wb, wv = self.win_tile(l, f"C_v{i}")
            self.projT(wb, wv, lambda tb, ps: self.cp(Hv[:, tb * 512:(tb + 1) * 512], ps[:, :], [ps], [Hv]))
            wb, wv = self.win_tile(l, f"C_o{i}")
            self.projT(wb, wv, lambda tb, ps: self.act(Hso[:, tb * 512:(tb + 1) * 512], ps[:, :], AF.Sigmoid, [ps], [Hso]))
            va = vaug_s.t[:, :].bitcast(BF16).rearrange("p (t h c) -> p t h c", h=2, c=128)
            self.memset(va[:, :, :, 64:128], 1.0, [vaug_s])
            idb = self.cst["ident"]
            for g4 in range(4):
                ps = self.nps()
                pb = ps.t[:, :].bitcast(BF16)
                for j in range(4):
                    tt_ = g4 * 4 + j
                    self.tr(pb[:, j * 128:(j + 1) * 128], Hv[:, tt_ * 128:(tt_ + 1) * 128], idb[:, :], [Hv, idb], [ps])
                self.cp(va[:, g4 * 4:(g4 + 1) * 4, :, 0:64],
                        pb[:, 0:512].rearrange("p (t h c) -> p t h c", h=2, c=64), [ps], [vaug_s])

            def out_fn(hh, g4, ps, hT=hT):
                sl = slice(g4 * 512, (g4 + 1) * 512)
                den = self.ta()
                self.act(den[0:64, :], ps[64:128, :], AF.Abs, [ps], [den])
                self.ts(den[0:64, :], den[0:64, :], 1.0, None, ALU.max, None, [den], [den])
                self.act(den[0:64, :], den[0:64, :], AF.Ln, [den], [den])
                self.act(den[0:64, :], den[0:64, :], AF.Exp, [den], [den], scale=-1.0)
                self.tt(den[0:64, :], ps[0:64, :], den[0:64, :], ALU.mult, [ps, den], [den])
                self.cp(hT[hh * 64:hh * 64 + 64, sl], den[0:64, :], [den], [hT])

            self.gla(qF, kF, GF, Dt, Et, Hq, Hk, Hqh, Hkh, khtm,
                     lambda tt_, hh: va[:, tt_, hh, :],
                     lambda tt_, half, hh: va[half * 64:half * 64 + 64, tt_, hh, :],
                     [vaug_s], 128, out_fn, (FS[6], FS[8]))
            self.tt(self.yC[i][:, :], hT[:, :], Hso[:, :], ALU.mult, [hT, Hso], [self.yC[i]])

    def attn(self, l):
        FS, HS = self.FS, self.HS
        cos, sin = HS[3], HS[4]
        for nm_, tt_ in (("cos", cos), ("sin", sin)):
            self.P.dma("pool", tt_[:, :], self.dr["cst"][:, CST_OFF[nm_]:CST_OFF[nm_] + T], writes=[tt_])
        num, den = FS[0], FS[1]
        zr, zp = FS[2], FS[3]
        mk = self.cst["m_att"]
        opad = self.cst["opad"]
        op3 = opad[:, :].rearrange("p (j c) -> p j c", c=128)
        for g in range(3):
            d = (1, 4, 16)[g]
            Hq, Hk, Hv = HS[0], HS[1], HS[2]
            vpad_s = FS[4]
            rot = self.cst["rot"]
            qb = HS[5]
            for nm, dstH in (("q", Hq), ("k", Hk)):
                wb, wv = self.win_tile(l, f"B_{nm}{g}")

                def ev(tb, ps):
                    self.cp(zr[:, tb * 512:(tb + 1) * 512], ps[:, :], [ps], [zr])
                    self.cp(qb[:, tb * 512:(tb + 1) * 512], ps[:, :], [ps], [qb], eng="dve")

                self.projT(wb, wv, ev)
                for tb in range(4):
                    sl = slice(tb * 512, (tb + 1) * 512)
                    ps = self.nps()
                    self.mm(ps[:, :], rot[:, :], qb[:, sl], True, True, [rot, qb], [ps])
                    self.tt(zp[:, sl], ps[:, :], sin[:, sl], ALU.mult, [ps, sin], [zp])
                self.tt(zr[:, :], zr[:, :], cos[:, :], ALU.mult, [zr, cos], [zr])
                self.tt(dstH[:, :], zr[:, :], zp[:, :], ALU.add, [zr, zp], [dstH])
            wb, wv = self.win_tile(l, f"B_v{g}")
            self.projT(wb, wv, lambda tb, ps: self.cp(Hv[:, tb * 512:(tb + 1) * 512], ps[:, :], [ps], [Hv]))
            vp = vpad_s.t[:, :].bitcast(BF16).rearrange("p (b j c) -> p b j c", j=2, c=128)
            if g == 0:
                self.memset(vp[:, :, 0, 64:128], 0.0, [vpad_s])
                self.memset(vp[:, :, 1, 0:64], 0.0, [vpad_s])
            nb = 16 // d
            idb = self.cst["ident"]

            def cols(r, nbk):
                st = nbk * 128 * d + r
                return slice(st, st + 127 * d + 1, d)

            blks = [(r, nbk) for r in range(d) for nbk in range(nb)]
            for g4 in range(4):
                ps = self.nps()
                pb = ps.t[:, :].bitcast(BF16)
                for j in range(4):
                    r, nbk = blks[g4 * 4 + j]
                    self.tr(pb[:, j * 128:(j + 1) * 128], Hv[:, cols(r, nbk)], idb[:, :], [Hv, idb], [ps])
                src = pb[:, 0:512].rearrange("p (b j c) -> p b j c", j=2, c=64)
                self.cp(vp[:, g4 * 4:(g4 + 1) * 4, 0, 0:64], src[:, :, 0, :], [ps], [vpad_s])
                self.cp(vp[:, g4 * 4:(g4 + 1) * 4, 1, 64:128], src[:, :, 1, :], [ps], [vpad_s], eng="dve")
            def scores(bi):
                r, nbk = blks[bi]
                hasprev = nbk > 0
                pt = self.tb()
                for hh in range(2):
                    po = hh * 64
                    pss = self.nps()
                    wdt = 256 if hasprev else 128
                    self.mm(pss[:, 0:128], Hk[po:po + 64, cols(r, nbk)],
                            Hq[po:po + 64, cols(r, nbk)], True, True, [Hk, Hq], [pss])
                    if hasprev:
                        self.mm(pss[:, 128:256], Hk[po:po + 64, cols(r, nbk - 1)],
                                Hq[po:po + 64, cols(r, nbk)], True, True, [Hk, Hq], [pss])
                    c1 = hh * 256
                    self.act(pt[:, c1:c1 + wdt], pss[:, 0:wdt], AF.Exp, [pss], [pt], scale=0.125)
                    self.tt(pt[:, c1:c1 + wdt], pt[:, c1:c1 + wdt], mk[:, 0:wdt], ALU.mult, [pt, mk], [pt])
                return pt

            pts = {0: scores(0)}
            for b4 in range(4):
                psn, psd = self.PS[6], self.PS[7]
                for jb in range(4):
                    bi = b4 * 4 + jb
                    r, nbk = blks[bi]
                    hasprev = nbk > 0
                    if bi + 1 < 16:
                        pts[bi + 1] = scores(bi + 1)
                    pt = pts.pop(bi)
                    c0 = jb * 128
                    seq = []
                    for hh in range(2):
                        seq.append((hh, bi, (2 * hh) * 128))
                        if hasprev:
                            seq.append((hh, bi - 1, (2 * hh + 1) * 128))
                    for ii, (hh, vb, pc0) in enumerate(seq):
                        self.mm(psn[:, c0:c0 + 128], vp[:, vb, hh, :], pt[:, pc0:pc0 + 128], ii == 0, ii == len(seq) - 1,
                                [vpad_s, pt], [psn])
                    for ii, (hh, vb, pc0) in enumerate(seq):
                        self.mm(psd[:, c0:c0 + 128], op3[:, hh, :], pt[:, pc0:pc0 + 128], ii == 0, ii == len(seq) - 1,
                                [opad, pt], [psd])
                if d == 1:
                    dn = num[:, b4 * 512:(b4 + 1) * 512]
                    dd = den[:, b4 * 512:(b4 + 1) * 512]
                    sn, sd = psn[:, :], psd[:, :]
                elif d == 4:
                    dn = num[:, :].rearrange("p (n r) -> p r n", r=4)[:, b4, :]
                    dd = den[:, :].rearrange("p (n r) -> p r n", r=4)[:, b4, :]
                    sn, sd = psn[:, :], psd[:, :]
                else:
                    dn = num[:, :].rearrange("p (n r) -> p r n", r=16)[:, b4 * 4:(b4 + 1) * 4, :]
                    dd = den[:, :].rearrange("p (n r) -> p r n", r=16)[:, b4 * 4:(b4 + 1) * 4, :]
                    sn = psn[:, :].rearrange("p (r n) -> p r n", n=128)
                    sd = psd[:, :].rearrange("p (r n) -> p r n", n=128)
                if g == 0:
                    self.cp(dn, sn, [psn], [num])
                    self.cp(dd, sd, [psd], [den], eng="dve")
                else:
                    self.tt(dn, sn, dn, ALU.add, [psn, num], [num])
                    self.tt(dd, sd, dd, ALU.add, [psd, den], [den])
        self.act(den[:, :], den[:, :], AF.Ln, [den], [den])
        self.act(den[:, :], den[:, :], AF.Exp, [den], [den], scale=-1.0)
        self.tt(self.yB[0][:, :], num[:, :], den[:, :], ALU.mult, [num, den], [self.yB[0]])

    def rwkv(self, l):
        FS, HS = self.FS, self.HS
        P = self.P
        zm = FS[8]
        pcx = self.pcx
        self.memset(pcx[:, 2:3], 0.0, [pcx])

        def proj_lerp(name, dst, mucol):
            wb, wv = self.win_tile(l, name)
            self.projT(wb, wv, lambda tb, ps: self.cp(pcx[:, 3 + tb * 512:3 + (tb + 1) * 512], ps[:, :], [ps], [pcx]))
            self.tt(dst[:, :], pcx[:, 2:2 + T], pcx[:, 3:3 + T], ALU.subtract, [pcx], [dst])
            self.stt(dst[:, :], dst[:, :], mucol, pcx[:, 3:3 + T], ALU.mult, ALU.add, [dst, pcx, self.vec[l]], [dst])

        proj_lerp("A_m", zm, self.vcol(l, "mu_m", 0))
        zmb = HS[8]
        self.act(zmb[0:32, :], zm[0:32, :], AF.Tanh, [zm], [zmb])
        self.cp(zmb[32:64, :], zm[32:64, :], [zm], [zmb])
        self.act(zmb[64:128, :], zm[64:128, :], AF.Sigmoid, [zm], [zmb])
        misc = self.misc
        bd = self.cst["bd64"]
        mrw = self.cst["m_rw"]
        mrwn = self.cst["m_rwn"]
        idb = self.cst["ident"]
        for i in range(2):
            rF, kF, aF, kkF, BF_, B1, Dt, Et = FS[0], FS[1], FS[2], FS[3], FS[4], FS[5], FS[6], FS[7]
            Hv, Hr, Ha, Hb, Hk, Hr0, Ha0, Hx = HS[0], HS[1], HS[2], HS[3], HS[4], HS[5], HS[6], HS[7]
            proj_lerp(f"A_r{i}", rF, self.vcol(l, "mu_r", i))
            proj_lerp(f"A_k{i}", kF, self.vcol(l, "mu_k", i))
            proj_lerp(f"A_v{i}", Dt, self.vcol(l, "mu_v", i))
            self.cp(Hv[:, :], Dt[:, :], [Dt], [Hv])
            cs = slice(i * 128, (i + 1) * 128)
            for tb in range(4):
                sl = slice(tb * 512, (tb + 1) * 512)
                ps = self.nps()
                self.mm(ps[:, :], misc[0:32, cs], zmb[0:32, sl], True, True, [misc, zmb], [ps])
                self.act(BF_[:, sl], ps[:, :], AF.Sigmoid, [ps, self.vec[l]], [BF_], bias=self.vcol(l, "w0", i))
                ps = self.nps()
                self.mm(ps[:, :], misc[32:64, cs], zmb[32:64, sl], True, True, [misc, zmb], [ps])
                self.act(aF[:, sl], ps[:, :], AF.Sigmoid, [ps, self.vec[l]], [aF], bias=self.vcol(l, "a0", i))
            self.ts(BF_[:, :], BF_[:, :], -0.6065306597126334, None, ALU.mult, None, [BF_], [BF_])
            self.ts(kkF[:, :], kF[:, :], self.vcol(l, "k_k", i), None, ALU.mult, None, [kF, self.vec[l]], [kkF])
            for tb in range(4):
                sl = slice(tb * 512, (tb + 1) * 512)
                sq = self.tb()
                self.act(sq[:, :], kkF[:, sl], AF.Square, [kkF], [sq])
                ps = self.nps()
                self.mm(ps[:, :], bd[:, :], sq[:, :], True, True, [bd, sq], [ps])
                lnv = self.ta()
                self.act(lnv[:, :], ps[:, :], AF.Ln, [ps, self.small], [lnv], bias=self.small[:, 2:3])
                self.act(lnv[:, :], lnv[:, :], AF.Exp, [lnv], [lnv], scale=-0.5)
                self.tt(kkF[:, sl], kkF[:, sl], lnv[:, :], ALU.mult, [kkF, lnv], [kkF])
            self.ts(Dt[:, :], aF[:, :], -1.0, self.vcol(l, "k_a", i), ALU.add, ALU.mult, [aF, self.vec[l]], [Dt])
            self.stt(kF[:, :], Dt[:, :], 1.0, kF[:, :], ALU.add, ALU.mult, [Dt, kF], [kF])
            self.stt(Hx[:, :], rF[:, :], self.vcol(l, "r_k", i), kF[:, :], ALU.mult, ALU.mult, [rF, kF, self.vec[l]], [Hx])
            self.tt(aF[:, :], aF[:, :], kkF[:, :], ALU.mult, [aF, kkF], [aF])
            self.ts(kkF[:, :], kkF[:, :], -1.0, None, ALU.mult, None, [kkF], [kkF])
            onecol = self.small[:, 0:1]
            P.op("dve", lambda e: e.tensor_tensor_scan(out=B1[:, :], data0=onecol.to_broadcast([128, T]), data1=BF_[:, :],
                                                       initial=0.0, op0=ALU.mult, op1=ALU.add), [BF_, self.small], [B1])
            self.tt(BF_[:, :], B1[:, :], BF_[:, :], ALU.subtract, [B1, BF_], [BF_])
            Bc, Bm = B1, BF_
            chs = self.chs
            self.memset(chs[:, 0, 0:1], 0.0, [chs])
            self.cp(chs[:, 0, 1:16], Bc[:, 127:T - 128:128], [Bc], [chs], eng="dve")
            self.cp(chs[:, 1, 0:16], Bc[:, 127:T:128], [Bc], [chs], eng="dve")
            self.tt(chs[:, 3, 0:16], chs[:, 1, 0:16], chs[:, 0, 0:16], ALU.subtract, [chs], [chs])
            self.act(chs[:, 2, 0:16], chs[:, 3, 0:16], AF.Exp, [chs], [chs])
            B3 = Bc[:, :].rearrange("p (n l) -> p n l", l=128)
            M3 = Bm[:, :].rearrange("p (n l) -> p n l", l=128)
            D3 = Dt[:, :].rearrange("p (n l) -> p n l", l=128)
            bmid = B3[:, :, 63:64].to_broadcast([128, 16, 128])
            bp = chs[:, 0, 0:16].unsqueeze(2).to_broadcast([128, 16, 128])
            be = chs[:, 1, 0:16].unsqueeze(2).to_broadcast([128, 16, 128])
            AR = self.AR
            AR4 = AR[:, :].rearrange("p (n j t) -> p n j t", j=2, t=128)

            def expmul(dst_ap, srcF, X3, ref, sign, reads, writes):
                if sign > 0:
                    self.tt(D3, X3, ref, ALU.subtract, reads, [Dt])
                else:
                    self.tt(D3, ref, X3, ALU.subtract, reads, [Dt])
                self.act(Et[:, :], Dt[:, :], AF.Exp, [Dt], [Et])
                self.tt(dst_ap, srcF, Et[:, :].rearrange("p (n t) -> p n t", t=128), ALU.mult, [Et] + reads, writes)

            r3 = rF[:, :].rearrange("p (n t) -> p n t", t=128)
            na3 = kkF[:, :].rearrange("p (n t) -> p n t", t=128)
            b3 = aF[:, :].rearrange("p (n t) -> p n t", t=128)
            k3 = kF[:, :].rearrange("p (n t) -> p n t", t=128)
            h3 = lambda H: H[:, :].rearrange("p (n t) -> p n t", t=128)
            expmul(AR4[:, :, 1, :], r3, B3, bmid, +1, [Bc, rF], [AR])
            expmul(AR4[:, :, 0, :], na3, M3, bmid, +1, [Bm, Bc, kkF], [AR])
            self.tt(D3, bmid, B3, ALU.subtract, [Bc], [Dt])
            self.act(Et[:, :], Dt[:, :], AF.Exp, [Dt], [Et])
            self.tt(Hb[:, :], aF[:, :], Et[:, :], ALU.mult, [aF, Et], [Hb])
            self.tt(Hk[:, :], kF[:, :], Et[:, :], ALU.mult, [kF, Et], [Hk])
            expmul(h3(Hr0), r3, B3, bp, +1, [Bc, chs, rF], [Hr0])
            expmul(h3(Ha0), na3, M3, bp, +1, [Bm, chs, kkF], [Ha0])
            self.tt(D3, be, B3, ALU.subtract, [Bc, chs], [Dt])
            self.act(Et[:, :], Dt[:, :], AF.Exp, [Dt], [Et])
            self.tt(Hr[:, :], aF[:, :], Et[:, :], ALU.mult, [aF, Et], [Hr])
            self.tt(Ha[:, :], kF[:, :], Et[:, :], ALU.mult, [kF, Et], [Ha])
            bhtm, khtm, vtm = FS[0], FS[1], FS[2]
            tmv = lambda s: s.t[:, 0:1024].bitcast(BF16).rearrange("p (t c) -> p t c", c=128)
            self.to_tm(Hr, bhtm, tmv(bhtm))
            self.to_tm(Ha, khtm, tmv(khtm))
            self.to_tm(Hv, vtm, tmv(vtm))
            bh3, kh3, v3 = tmv(bhtm), tmv(khtm), tmv(vtm)
            yT = FS[3]
            Sb = self.Sb
            self.memset(Sb[:, 0, 0:64], 0.0, [Sb])
            self.memset(self.Sf[0][:, 0:64], 0.0, [self.Sf[0]])

            def sub_bufs(parent, n, nm_):
                out = []
                for j in range(n):
                    b = Buf(f"{nm_}{j}")
                    b.r = list(parent.b.r)
                    if parent.b.w is not None:
                        b.r.append(parent.b.w)
                    out.append(b)
                return out

            def merge_back(parent, bufs):
                for b in bufs:
                    parent.b.r.extend(b.r)
                    if b.w is not None:
                        parent.b.r.append(b.w)

            ATst, PTst, stparents = [], [], []
            for q in range(4):
                par = FS[4 + q]
                bl = sub_bufs(par, 8, f"ATst{q}_")
                stparents.append((par, bl))
                v = par.t[:, :].bitcast(BF16)
                for j in range(8):
                    ATst.append(TT(v[:, j * 512:(j + 1) * 512], bl[j]))
            PTg = []
            for q in range(2):
                par = FS[q]
                bl = sub_bufs(par, 4, f"PTst{q}_")
                stparents.append((par, bl))
                v = par.t[:, 1024:2048].bitcast(BF16)
                for j in range(16):
                    PTst.append(TT(v[:, j * 128:(j + 1) * 128], bl[j // 4]))
                for j in range(4):
                    PTg.append(TT(v[:, j * 512:(j + 1) * 512], bl[j]))
            self.nrot = 8
            ydt = self.ydt
            NN = [[TT(ydt.t[:, par * 1024 + h * 512: par * 1024 + (h + 1) * 512], Buf(f"NN{par}{h}")) for h in range(2)]
                  for par in range(2)]
            PTp = [[TT(ydt.t[:, 2048 + par * 512 + h * 256: 2048 + par * 512 + (h + 1) * 256], Buf(f"PTp{par}{h}"))
                    for h in range(2)] for par in range(2)]
            N0t = TT(ydt.t[:, 3072:3584], Buf("N0t"))
            mrwn4 = self.cst["m_rwn4"]
            self.nm_bufs.extend(NN[0] + NN[1] + PTp[0] + PTp[1] + [N0t])
            def score_stage(g8):
                items = [(2 * g8 + j // 2, j % 2) for j in range(4)]
                ps2h = [self.nps(), self.nps()]
                for s_, (n, hh) in enumerate(items):
                    po = hh * 64
                    cs_ = slice(n * 128, (n + 1) * 128)
                    it = n * 2 + hh
                    AT = ATst[it]
                    ps = self.nps()
                    arn = AR4[po:po + 64, n, :, :].rearrange("p j t -> p (j t)")
                    self.mm(ps[:, 0:256], Hb[po:po + 64, cs_], arn, True, True, [Hb, AR], [ps])
                    self.mm(ps[:, 256:512], Hk[po:po + 64, cs_], arn, True, True, [Hk, AR], [ps])
                    self.tt(AT[:, :], ps[:, :], mrw[:, :], ALU.mult, [ps, mrw], [AT])
                    self.mm(ps2h[hh][:, (s_ // 2) * 128:(s_ // 2 + 1) * 128], AR4[po:po + 64, n, 0, :], Hb[po:po + 64, cs_],
                            True, True, [AR, Hb], [ps2h[hh]])
                n0v = N0t[:, :].rearrange("p (j h x) -> p j h x", h=2, x=128)
                for hh in range(2):
                    self.tt(n0v[:, :, hh, :], ps2h[hh][:, 0:256].rearrange("p (j x) -> p j x", x=128),
                            mrwn4[:, 0:256].rearrange("p (j x) -> p j x", x=128), ALU.mult, [ps2h[hh], mrwn4], [N0t])

            score_stage(0)
            for g8 in range(8):
                items = [(2 * g8 + j // 2, j % 2) for j in range(4)]
                for s_, (n, hh) in enumerate(items):
                    AT = ATst[n * 2 + hh]
                    p0 = PTp[0][s_ // 2]
                    self.tt(p0[:, (s_ % 2) * 128:(s_ % 2 + 1) * 128], AT[:, 0:128], idb[:, :], ALU.add, [AT, idb], [p0])
                for lev in range(6):
                    par = lev % 2
                    for h in range(2):
                        psa = self.nps()
                        for q2 in range(2):
                            s_ = 2 * h + q2
                            n, hh = items[s_]
                            if lev == 0:
                                cN, cNT = N0t[:, s_ * 128:(s_ + 1) * 128], ATst[n * 2 + hh][:, 0:128]
                                rd = [N0t, ATst[n * 2 + hh]]
                            else:
                                src = NN[1 - par][h]
                                cN, cNT = src[:, q2 * 256:q2 * 256 + 128], src[:, q2 * 256 + 128:q2 * 256 + 256]
                                rd = [src]
                            self.mm(psa[:, q2 * 256:q2 * 256 + 128], cNT, cN, True, True, rd, [psa])
                            if lev < 5:
                                self.mm(psa[:, q2 * 256 + 128:q2 * 256 + 256], cN, cNT, True, True, rd, [psa])
                        dst = NN[par][h]
                        if lev < 5:
                            self.cp(dst[:, :], psa[:, :], [psa], [dst])
                        else:
                            self.cp(dst[:, :].rearrange("p (q x) -> p q x", x=256)[:, :, 0:128],
                                    psa[:, :].rearrange("p (q x) -> p q x", x=256)[:, :, 0:128], [psa], [dst])
                    for h in range(2):
                        psc = self.nps()
                        pcur = PTp[par][h]
                        for q2 in range(2):
                            nN = NN[par][h][:, q2 * 256:q2 * 256 + 128]
                            self.mm(psc[:, q2 * 128:(q2 + 1) * 128], nN, pcur[:, q2 * 128:(q2 + 1) * 128], True, True,
                                    [NN[par][h], pcur], [psc])
                        if lev == 5:
                            PT2 = TT(PTg[g8].t[:, h * 256:(h + 1) * 256], PTg[g8].b)
                        else:
                            PT2 = PTp[1 - par][h]
                        self.tt(PT2[:, :], psc[:, 0:256], pcur[:, :], ALU.add, [psc, pcur], [PT2])
                    if lev == 0 and g8 + 1 < 8:
                        score_stage(g8 + 1)
            self.nrot = 6
            self.psi = self.psi % 6
            for n in range(16):
                cs_ = slice(n * 128, (n + 1) * 128)
                Us = []
                psxs = []
                for hh in range(2):
                    po = hh * 64
                    AT = ATst[n * 2 + hh]
                    psx = self.nps()
                    self.mm(psx[:, 0:64], AT[:, 256:384], v3[:, n, po:po + 64], True, False, [AT, vtm], [psx])
                    self.mm(psx[:, 0:64], Ha0[po:po + 64, cs_], Sb[po:po + 64, n, 0:64], False, True, [Ha0, Sb], [psx])
                    psxs.append(psx)
                for hh in range(2):
                    X = self.xu[2 * hh]
                    self.cp(X[:, 0:64], psxs[hh][:, 0:64], [psxs[hh]], [X], eng=("act" if hh == 0 else "dve"))
                psus = []
                for hh in range(2):
                    PT = PTst[n * 2 + hh]
                    X = self.xu[2 * hh]
                    psu = self.nps()
                    self.mm(psu[:, 0:64], PT[:, 0:128], X[:, 0:64], True, True, [PT, X], [psu])
                    psus.append(psu)
                for hh in range(2):
                    U = self.xu[2 * hh + 1]
                    self.cp(U[:, 0:64], psus[hh][:, 0:64], [psus[hh]], [U], eng=("dve" if hh == 0 else "act"))
                    Us.append(U)
                psS = self.nps()
                for hh in range(2):
                    po = hh * 64
                    U = Us[hh]
                    self.mm(psS[po:po + 64, 0:64], bh3[:, n, po:po + 64], U[:, 0:64], True, False, [bhtm, U], [psS])
                    self.mm(psS[po:po + 64, 0:64], kh3[:, n, po:po + 64], v3[:, n, po:po + 64], False, True, [khtm, vtm], [psS])
                sfo, sfn = self.Sf[n % 2], self.Sf[(n + 1) % 2]
                self.stt(sfn[:, 0:64], sfo[:, 0:64], chs[:, 2, n:n + 1], psS[:, 0:64], ALU.mult, ALU.add, [sfo, chs, psS], [sfn])
                self.cp(Sb[:, n + 1, 0:64], sfn[:, 0:64], [sfn], [Sb])
                for hh in range(2):
                    po = hh * 64
                    AT, U = ATst[n * 2 + hh], Us[hh]
                    psy = self.nps()
                    self.mm(psy[0:64, 0:128], v3[:, n, po:po + 64], AT[:, 384:512], True, False, [vtm, AT], [psy])
                    self.mm(psy[0:64, 0:128], U[:, 0:64], AT[:, 128:256], False, False, [U, AT], [psy])
                    self.mm(psy[0:64, 0:128], Sb[po:po + 64, n, 0:64], Hr0[po:po + 64, cs_], False, True, [Sb, Hr0], [psy])
                    self.cp(yT[po:po + 64, cs_], psy[0:64, 0:128], [psy], [yT])
            for par, bl in stparents:
                merge_back(par, bl)
            for tb in range(4):
                sl = slice(tb * 512, (tb + 1) * 512)
                yb = self.tb()
                self.cp(yb[:, :], yT[:, sl], [yT], [yb])
                ps = self.nps()
                self.mm(ps[:, :], bd[:, :], yb[:, :], True, True, [bd, yb], [ps])
                yc = self.ta()
                self.stt(yc[:, :], ps[:, :], -1.0 / 64, yT[:, sl], ALU.mult, ALU.add, [ps, yT], [yc])
                sq = self.tb()
                self.act(sq[:, :], yc[:, :], AF.Square, [yc], [sq])
                ps2 = self.nps()
                self.mm(ps2[:, :], bd[:, :], sq[:, :], True, True, [bd, sq], [ps2])
                lnv = self.ta()
                self.act(lnv[:, :], ps2[:, :], AF.Ln, [ps2, self.small], [lnv], scale=1.0 / 64, bias=self.small[:, 3:4])
                self.act(lnv[:, :], lnv[:, :], AF.Exp, [lnv], [lnv], scale=-0.5)
                self.tt(yc[:, :], yc[:, :], lnv[:, :], ALU.mult, [yc, lnv], [yc])
                self.ts(yc[:, :], yc[:, :], self.vcol(l, "ln_g", i), self.vcol(l, "ln_b", i), ALU.mult, ALU.add,
                        [yc, self.vec[l]], [yc])
                ps3 = self.nps()
                self.mm(ps3[:, :], bd[:, :], Hx[:, sl], True, True, [bd, Hx], [ps3])
                bo = self.ta()
                self.tt(bo[:, :], ps3[:, :], Hv[:, sl], ALU.mult, [ps3, Hv], [bo])
                self.tt(yc[:, :], yc[:, :], bo[:, :], ALU.add, [yc, bo], [yc])
                ps4 = self.nps()
                self.mm(ps4[:, :], misc[64:128, cs], zmb[64:128, sl], True, True, [misc, zmb], [ps4])
                self.tt(self.yA[i][:, sl], ps4[:, :], yc[:, :], ALU.mult, [ps4, yc], [self.yA[i]])
        for t_ in self.nm_bufs + self.xu:
            for yb_ in self.yD:
                yb_.b.r.extend(t_.b.r)
                if t_.b.w is not None:
                    yb_.b.r.append(t_.b.w)
        self.nm_bufs = []

    def merge_wout(self, l):
        P = self.P
        ys = [(self.yA, 0, 2), (self.yB, 2, 1), (self.yC, 3, 2), (self.yD, 5, 2)]
        mg = self.HS[0:8]
        acc = self.FS[8]
        for dt in range(8):
            for b in range(4):
                wb, wv = self.win_tile(l, f"G_{b}_{dt}")
                yl, k0, nk = ys[b]
                psrc = self.dr["pall"][l, k0 * 128:(k0 + nk) * 128, dt * 128:(dt + 1) * 128].rearrange("(k p) m -> p k m", p=128)
                pwb, pwv = self.wload(psrc, nk, 128)
                for tb in range(4):
                    sl = slice(tb * 512, (tb + 1) * 512)
                    ps = self.nps()
                    for kc in range(8):
                        self.mm(ps[:, :], wv[:, kc, :], self.xnT[:, kc, sl], kc == 0, kc == 7, [wb, self.xnT], [ps])
                    gs = self.ta()
                    self.act(gs[:, :], ps[:, :], AF.Sigmoid, [ps, self.vec[l]], [gs], bias=self.vcol(l, "bgate", b * 8 + dt))
                    pp = self.nps()
                    for kc in range(nk):
                        self.mm(pp[:, :], pwv[:, kc, :], yl[kc][:, sl], kc == 0, kc == nk - 1,
                                [pwb, yl[kc]], [pp])
                    if b == 0:
                        self.tt(acc[:, sl], pp[:, :], gs[:, :], ALU.mult, [pp, gs], [acc])
                    else:
                        self.tt(gs[:, :], pp[:, :], gs[:, :], ALU.mult, [pp, gs], [gs])
                        if b < 3:
                            self.tt(acc[:, sl], acc[:, sl], gs[:, :], ALU.add, [acc, gs], [acc])
                        else:
                            self.tt(mg[dt][:, sl], acc[:, sl], gs[:, :], ALU.add, [acc, gs], [mg[dt]])
        for kc in range(8):
            P.dma("sp", self.FS[kc][:, :], self.dr["hscr"][kc], reads=[self.hb[kc]], writes=[self.FS[kc]])
        for dt in range(8):
            src = self.dr["w_out"][l, :, dt * 128:(dt + 1) * 128].rearrange("(k p) m -> p k m", p=128)
            wb, wv = self.wload(src, 8, 128)
            for tb in range(4):
                sl = slice(tb * 512, (tb + 1) * 512)
                ps = self.nps()
                for kc in range(8):
                    self.mm(ps[:, :], wv[:, kc, :], mg[kc][:, sl], kc == 0, kc == 7, [wb, mg[kc]], [ps])
                self.tt(self.FS[dt][:, sl], ps[:, :], self.FS[dt][:, sl], ALU.add, [ps, self.FS[dt]], [self.FS[dt]])

    def ffn(self, l):
        FS, HS = self.FS, self.HS
        groups = [list(range(0, 6)), list(range(6, 12)), list(range(12, 17)), list(range(17, 22))]
        stgs = {"u": TT(self.pcx.t[:, 0:T], self.pcx.b), "g": TT(self.AR.t[:, :].bitcast(F32), self.AR.b)}
        cu, cg = FS[8], self.cg

        def up_tile(j):
            for which, dstF, cidx in (("u", cu, j), ("g", cg, NFT + j)):
                stg = stgs[which]
                col0 = (0 if which == "u" else DFF) + j * 128
                src = self.dr["w_up"][l, :, col0:col0 + 128].rearrange("(k p) m -> p k m", p=128)
                wb, wv = self.wload(src, 8, 128)
                self.projT(wb, wv, lambda tb, ps, stg=stg: self.cp(stg[:, tb * 512:(tb + 1) * 512], ps[:, :], [ps], [stg]))
                self.act(dstF[:, :], stg[:, 0:T], AF.Identity, [stg, self.vec[l]], [dstF],
                         scale=self.vcol(l, "fcw2", cidx), bias=self.vcol(l, "fcb", cidx))
                self.stt(dstF[:, 1:T], stg[:, 0:T - 1], self.vcol(l, "fcw1", cidx), dstF[:, 1:T], ALU.mult, ALU.add,
                         [stg, dstF, self.vec[l]], [dstF])
                self.stt(dstF[:, 2:T], stg[:, 0:T - 2], self.vcol(l, "fcw0", cidx), dstF[:, 2:T], ALU.mult, ALU.add,
                         [stg, dstF, self.vec[l]], [dstF])
            self.act(cg[:, :], cg[:, :], AF.Silu, [cg], [cg])
            hs = HS[j % 9]
            self.tt(hs[:, :], cg[:, :], cu[:, :], ALU.mult, [cg, cu], [hs])

        def down_group(G):
            ng = len(G)
            for dt in range(8):
                src = self.dr["w_down"][l, G[0] * 128:(G[0] + ng) * 128, dt * 128:(dt + 1) * 128].rearrange("(k p) m -> p k m", p=128)
                wb, wv = self.wload(src, ng, 128)
                for tb in range(4):
                    sl = slice(tb * 512, (tb + 1) * 512)
                    ps = self.nps()
                    for gi, j in enumerate(G):
                        hs = HS[j % 9]
                        self.mm(ps[:, :], wv[:, gi, :], hs[:, sl], gi == 0, gi == ng - 1, [wb, hs], [ps])
                    self.tt(FS[dt][:, sl], ps[:, :], FS[dt][:, sl], ALU.add, [ps, FS[dt]], [FS[dt]])

        prev = None
        for G in groups:
            for gi, j in enumerate(G):
                up_tile(j)
                if gi == 1 and prev is not None:
                    down_group(prev)
                    prev = None
            prev = G
        down_group(prev)

    def layer_consts(self, l):
        P = self.P
        sm = self.small
        self.memset(sm[:, 1:2], EPS, [sm])
        self.memset(sm[:, 2:3], 1e-12, [sm])
        self.memset(sm[:, 3:4], 64e-5, [sm])
        for i in range(2):
            if l == 0:
                self.memset(sm[:, 4 + i:5 + i], 0.0, [sm])
                self.memset(sm[:, 6 + i:7 + i], 1.0, [sm])
            else:
                self.tt(sm[:, 10 + i:11 + i], self.vcol(l, "lb1", i), self.vcol(l, "lb0", i), ALU.subtract, [self.vec[l]], [sm])
                self.act(sm[:, 4 + i:5 + i], sm[:, 10 + i:11 + i], AF.Sigmoid, [sm], [sm])
                self.ts(sm[:, 6 + i:7 + i], sm[:, 4 + i:5 + i], -1.0, 1.0, ALU.mult, ALU.add, [sm], [sm])
            self.ts(sm[:, 8 + i:9 + i], self.vcol(l, "f_b", i), -1.0, None, ALU.mult, None, [self.vec[l]], [sm])
        P.dma("pool", self.misc[:, :], self.dr["miscw"][l], writes=[self.misc])

    def run(self, stages):
        P = self.P
        self.setup()
        self.load_x()
        di = 0
        for l in range(DEPTH):
            self.layer_consts(l)
            self.rmsnorm(l, "nmg")
            for kc in range(8):
                P.dma("sp", self.dr["hscr"][kc], self.FS[kc][:, :], reads=[self.FS[kc]], writes=[self.hb[kc]])
            if "A" in stages:
                self.rwkv(l)
            else:
                for t_ in self.yA:
                    self.memset(t_[:, :], 0.0, [t_])
            if "B" in stages:
                self.attn(l)
            if "C" in stages:
                self.mlstm(l)
            if "D" in stages:
                self.hgrn2(l)
            if self.dbg is not None and l == 0:
                for t_ in self.yA + self.yB + self.yC + self.yD:
                    self.dump(di, t_, t_[:, :])
                    di += 1
            if "M" not in stages:
                break
            self.nrot = 8
            self.merge_wout(l)
            self.rmsnorm(l, "nfg")
            self.ffn(l)
            if l + 1 < DEPTH:
                self.nrot = 6
                self.psi = self.psi % 6
        if "M" in stages:
            toks = []

            ones = self.cst["ones"]
            for tb in range(4):
                sl = slice(tb * 512, (tb + 1) * 512)
                ps = self.nps()
                for kc in range(8):
                    sq = self.tb()
                    self.act(sq[:, :], self.FS[kc][:, sl], AF.Square, [self.FS[kc]], [sq])
                    self.mm(ps[:, :], ones[:, :], sq[:, :], kc == 0, kc == 7, [ones, sq], [ps])
                lnv = self.ta()
                self.act(lnv[:, :], ps[:, :], AF.Ln, [ps, self.small], [lnv], scale=1.0 / D, bias=self.small[:, 1:2])
                rstd = TT(self.pcx.t[:, 0:512], self.pcx.b)
                self.act(rstd[:, :], lnv[:, :], AF.Exp, [lnv], [rstd], scale=-0.5)
                for kc in range(8):
                    self.stt(self.FS[kc][:, sl], self.FS[kc][:, sl], self.vcol(0, "fing", kc), rstd[:, :],
                             ALU.mult, ALU.mult, [self.FS[kc], rstd, self.vec[0]], [self.FS[kc]])
                for j in range(4):
                    stg = self.HS[j % 2]
                    stgv = stg.t[:, :].bitcast(F32)
                    cs_ = slice(tb * 512 + j * 128, tb * 512 + (j + 1) * 128)
                    for half in range(2):
                        pst = self.nps()
                        for q4 in range(4):
                            kc = half * 4 + q4
                            self.tr(pst[:, q4 * 128:(q4 + 1) * 128], self.FS[kc][:, cs_], self.identF[:, :],
                                    [self.FS[kc], self.identF], [pst])
                        self.cp(stgv[:, half * 512:(half + 1) * 512], pst[:, :], [pst], [stg],
                                eng=("act" if half == 0 else "dve"))
                    r0 = tb * 512 + j * 128
                    toks.append(P.dma("sp", self.dr["out"][r0:r0 + 128, :], stgv, reads=[stg]))
            P.finish_wait("sp", toks)
        if self.dbg_toks:
            P.finish_wait("pool", self.dbg_toks)


def build(stages="ABCDM", debug=False):
    nc = bass.Bass("TRN2", target_bir_lowering=False)
    dr = {}
    dr["x"] = nc.dram_tensor("x", [T, D], F32, kind="ExternalInput").ap()
    dr["w_in"] = nc.dram_tensor("w_in", [DEPTH, D, NWT * 128], F32, kind="ExternalInput").ap()
    dr["vecs"] = nc.dram_tensor("vecs", [DEPTH, 128, NVEC], F32, kind="ExternalInput").ap()
    dr["cst"] = nc.dram_tensor("cst", [128, NCST], F32, kind="ExternalInput").ap()
    dr["miscw"] = nc.dram_tensor("miscw", [DEPTH, 128, 256], F32, kind="ExternalInput").ap()
    dr["pall"] = nc.dram_tensor("pall", [DEPTH, 896, D], F32, kind="ExternalInput").ap()
    dr["w_out"] = nc.dram_tensor("w_out", [DEPTH, D, D], F32, kind="ExternalInput").ap()
    dr["w_up"] = nc.dram_tensor("w_up", [DEPTH, D, 2 * DFF], F32, kind="ExternalInput").ap()
    dr["w_down"] = nc.dram_tensor("w_down", [DEPTH, DFF, D], F32, kind="ExternalInput").ap()
    dr["out"] = nc.dram_tensor("out", [T, D], F32, kind="ExternalOutput").ap()
    dr["hscr"] = nc.dram_tensor("hscr", [8, 128, T], F32, kind="Internal").ap()
    dbg = None
    if debug:
        dbg = nc.dram_tensor("dbg", [8, 128, T], F32, kind="ExternalOutput").ap()
    with ExitStack() as es:
        P = Prog(nc, es)
        K = Kern(nc, P, dr, dbg)
        K.run(stages)
        P.emit()
        print(f"[build] ops={P.n_ops} waits={P.n_wait} counts={P.count}")
    return nc


def host_inputs(inp):
    inp = {k: np.asarray(v) for k, v in inp.items()}
    shared = {}
    shared["w_in"] = np.ascontiguousarray(inp["w_in"][:, :, WIN_COLS])
    shared["vecs"] = np.stack([_pack_vecs(inp, l) for l in range(DEPTH)])
    shared["cst"] = _consts()
    shared["miscw"] = np.ascontiguousarray(np.concatenate([inp["rwkv_w2"], inp["rwkv_a2"], inp["rwkv_g2"]], axis=1))
    shared["pall"] = np.ascontiguousarray(np.concatenate([inp["p_rwkv"], inp["p_attn"], inp["p_mlstm"], inp["p_hgrn"]], axis=1))
    shared["w_out"] = np.ascontiguousarray(inp["w_out"])
    shared["w_up"] = np.ascontiguousarray(inp["w_up"])
    shared["w_down"] = np.ascontiguousarray(inp["w_down"])
    return shared


_NC_CACHE = {}


def kernel(**inputs):
    shared = host_inputs(inputs)
    x = np.asarray(inputs["x"], np.float32)
    if "nc" not in _NC_CACHE:
        _NC_CACHE["nc"] = build("ABCDM", False)
    nc = _NC_CACHE["nc"]
    in_maps = []
    for c in range(8):
        m = dict(shared)
        m["x"] = np.ascontiguousarray(x[c])
        in_maps.append(m)
    res = run_bass_kernel_spmd(nc, in_maps, core_ids=list(range(8)))
    return np.stack([np.asarray(r["out"], np.float32) for r in res.results], axis=0)
```

```python
import numpy as np
from contextlib import ExitStack
import concourse.bass as bass
import concourse.mybir as mybir
from concourse.bass_utils import run_bass_kernel_spmd

F32 = mybir.dt.float32
BF16 = mybir.dt.bfloat16
AF = mybir.ActivationFunctionType
ALU = mybir.AluOpType

T = 2048
D = 1024
DEPTH = 2
NTT = 16
DFF = 2816
NFT = 22
N_IN = 8200
EPS = 1e-6


class Buf:
    __slots__ = ("w", "r", "name", "psum")

    def __init__(self, name="", psum=False):
        self.w = None
        self.r = []
        self.name = name
        self.psum = psum


class TT:
    def __init__(self, t, b):
        self.t = t
        self.b = b

    def __getitem__(self, k):
        return self.t[k]


class Prog:
    ENG = ("pe", "act", "dve", "pool", "sp")
    NDMA = 8

    def __init__(self, nc, es):
        self.nc = nc
        self.es = es
        self.ops = {e: [] for e in self.ENG}
        self.count = {e: 0 for e in self.ENG}
        self.epoch = {e: 0 for e in self.ENG}
        self.sem = {e: es.enter_context(nc.semaphore("sem_" + e)) for e in self.ENG if e != "sp"}
        self.known = {e: {} for e in self.ENG}
        self.dsem = {}
        self.dcount = {}
        self.dnext = {}
        for q in ("sp", "act", "pool"):
            self.dsem[q] = [es.enter_context(nc.semaphore(f"dsem_{q}{i}")) for i in range(self.NDMA)]
            self.dcount[q] = [0] * self.NDMA
            self.dnext[q] = 0
        self.semobj = dict(self.sem)
        for q in self.dsem:
            for i, s in enumerate(self.dsem[q]):
                self.semobj[("d", q, i)] = s
        self.n_wait = 0
        self.n_ops = 0

    def sb(self, name, shape, dt=F32):
        t = self.es.enter_context(self.nc.sbuf_tensor(name, list(shape), dt))
        return TT(t, Buf(name))

    def ps(self, name, shape, dt=F32):
        t = self.es.enter_context(self.nc.psum_tensor(name, list(shape), dt))
        return TT(t, Buf(name, psum=True))

    @staticmethod
    def _bl(xs):
        out = []
        for x in xs:
            if isinstance(x, TT):
                if isinstance(x.b, list):
                    out.extend(x.b)
                else:
                    out.append(x.b)
            elif isinstance(x, (list, tuple)):
                out.extend(Prog._bl(x))
            else:
                out.append(x)
        return out

    def _deps(self, reads, writes):
        deps = {}

        def add(tok):
            k, v = tok
            if v > deps.get(k, 0):
                deps[k] = v

        for b in reads:
            if b.w is not None:
                add(b.w)
            if b.psum:
                for r in b.r:
                    add(r)
        for b in writes:
            if b.w is not None:
                add(b.w)
            for r in b.r:
                add(r)
        return deps

    def _commit(self, reads, writes, tok):
        for b in reads:
            b.r.append(tok)
            if len(b.r) > 64:
                mx = {}
                for k, v in b.r:
                    if v > mx.get(k, 0):
                        mx[k] = v
                b.r = list(mx.items())
        for b in writes:
            b.w = tok
            b.r = []

    EPOCH = 8000

    def _ekey(self, e):
        ep = self.epoch[e]
        return e if ep == 0 else f"{e}#{ep}"

    def op(self, e, fn, reads=(), writes=()):
        reads = self._bl(reads)
        writes = self._bl(writes)
        if self.count[e] >= self.EPOCH:
            self.epoch[e] += 1
            self.count[e] = 0
            nk = self._ekey(e)
            self.semobj[nk] = self.es.enter_context(self.nc.semaphore("sem_" + nk.replace("#", "_")))
        ek = self._ekey(e)
        idx = self.count[e] + 1
        deps = self._deps(reads, writes)
        waits = []
        kn = self.known[e]
        for k, v in deps.items():
            kb = k.split("#")[0] if isinstance(k, str) else None
            if kb == e:
                if e == "pe":
                    continue
                if k == ek and v <= idx - 6:
                    continue
            if v > kn.get(k, 0):
                waits.append((k, v))
                kn[k] = v
        self.count[e] = idx
        tok = (ek, idx)
        self.ops[e].append((waits, fn, (ek, 1)))
        self.n_wait += len(waits)
        self.n_ops += 1
        self._commit(reads, writes, tok)
        return tok

    def dma(self, q, out, in_, reads=(), writes=()):
        reads = self._bl(reads)
        writes = self._bl(writes)
        j = self.dnext[q]
        self.dnext[q] = (j + 1) % self.NDMA
        key = ("d", q, j)
        prev = self.dcount[q][j]
        deps = self._deps(reads, writes)
        if prev > 0:
            deps[key] = max(deps.get(key, 0), 16 * prev)
        waits = []
        kn = self.known[q]
        for k, v in deps.items():
            if v > kn.get(k, 0):
                waits.append((k, v))
                kn[k] = v
        self.dcount[q][j] = prev + 1
        tok = (key, 16 * (prev + 1))

        def fn(eng):
            return eng.dma_start(out=out, in_=in_)

        self.ops[q].append((waits, fn, (key, 16)))
        self.n_wait += len(waits)
        self.n_ops += 1
        self._commit(reads, writes, tok)
        return tok

    def finish_wait(self, e, toks):
        self.ops[e].append((list(toks), None, None))

    def emit(self):
        nc = self.nc
        with nc.Block() as block:
            def run(ename, eng):
                for waits, fn, inc in self.ops[ename]:
                    for k, v in waits:
                        eng.wait_ge(self.semobj[k], v)
                    if fn is not None:
                        ins = fn(eng)
                        ins.then_inc(self.semobj[inc[0]], inc[1])

            @block.sync
            def _(eng):
                run("sp", eng)

            @block.scalar
            def _(eng):
                run("act", eng)

            @block.vector
            def _(eng):
                run("dve", eng)

            @block.gpsimd
            def _(eng):
                run("pool", eng)

            @block.tensor
            def _(eng):
                run("pe", eng)


OA, OB, OC, OD, OG = 0, 896, 2048, 3080, 4104


def _win_cols():
    tiles = {}
    cols = []

    def add(name, idx):
        idx = np.asarray(idx, dtype=np.int64)
        assert idx.size == 128
        tiles[name] = len(cols)
        cols.append(idx)

    ar = np.arange
    for i in range(2):
        add(f"A_r{i}", OA + i * 128 + ar(128))
        add(f"A_k{i}", OA + 256 + i * 128 + ar(128))
        add(f"A_v{i}", OA + 512 + i * 128 + ar(128))
    add("A_m", OA + 768 + ar(128))
    perm = np.concatenate([ar(32) + 32, ar(32)])
    perm128 = np.concatenate([perm, perm + 64])
    for g in range(3):
        add(f"B_q{g}", OB + g * 128 + ar(128))
        add(f"B_k{g}", OB + 384 + g * 128 + ar(128))
        add(f"B_v{g}", OB + 768 + g * 128 + ar(128))
    for i in range(2):
        add(f"C_q{i}", OC + i * 128 + ar(128))
        add(f"C_k{i}", OC + 256 + i * 128 + ar(128))
        add(f"C_v{i}", OC + 512 + i * 128 + ar(128))
        add(f"C_o{i}", OC + 768 + i * 128 + ar(128))
        add(f"C_ig{i}", OC + 1024 + np.repeat(ar(2) + 2 * i, 64))
        add(f"C_fg{i}", OC + 1028 + np.repeat(ar(2) + 2 * i, 64))
    for i in range(2):
        add(f"D_q{i}", OD + i * 128 + ar(128))
        add(f"D_f{i}", OD + 256 + i * 128 + ar(128))
        add(f"D_i{i}", OD + 512 + i * 128 + ar(128))
        add(f"D_g{i}", OD + 768 + i * 128 + ar(128))
    for b in range(4):
        for dt in range(8):
            add(f"G_{b}_{dt}", OG + b * 1024 + dt * 128 + ar(128))
    return tiles, np.concatenate(cols)


WIN_TILES, WIN_COLS = _win_cols()
NWT = len(WIN_TILES)

VEC_SPEC = [
    ("nmg", 1024), ("nfg", 1024), ("bgate", 4096),
    ("mu_r", 256), ("mu_k", 256), ("mu_v", 256), ("mu_m", 128),
    ("w0", 256), ("a0", 256), ("k_k", 256), ("k_a", 256), ("r_k", 256), ("ln_g", 256), ("ln_b", 256),
    ("mcw0", 512), ("mcw1", 512), ("mcw2", 512), ("mcw3", 512), ("mcb", 512),
    ("i_b", 256), ("f_b", 256), ("lb0", 256), ("lb1", 256), ("hng", 256),
    ("fcw0", 5632), ("fcw1", 5632), ("fcw2", 5632), ("fcb", 5632), ("fing", 1024),
]
VEC_OFF = {}
_o = 0
for _n, _l in VEC_SPEC:
    VEC_OFF[_n] = _o
    _o += _l // 128
NVEC = _o


def _pack_vecs(inp, l):
    v = {
        "nmg": inp["norm_mix_g"][l], "nfg": inp["norm_ffn_g"][l], "bgate": inp["b_gate"][l].reshape(-1),
        "mu_r": inp["rwkv_mu"][l][0:256], "mu_k": inp["rwkv_mu"][l][256:512], "mu_v": inp["rwkv_mu"][l][512:768],
        "mu_m": inp["rwkv_mu"][l][768:896],
        "w0": inp["rwkv_w0"][l], "a0": inp["rwkv_a0"][l], "k_k": inp["rwkv_k_k"][l], "k_a": inp["rwkv_k_a"][l],
        "r_k": inp["rwkv_r_k"][l].reshape(-1), "ln_g": inp["rwkv_ln_g"][l], "ln_b": inp["rwkv_ln_b"][l],
        "mcw0": inp["mlstm_conv_w"][l][0], "mcw1": inp["mlstm_conv_w"][l][1], "mcw2": inp["mlstm_conv_w"][l][2],
        "mcw3": inp["mlstm_conv_w"][l][3], "mcb": inp["mlstm_conv_b"][l],
        "i_b": np.repeat(inp["mlstm_i_b"][l], 64), "f_b": np.repeat(inp["mlstm_f_b"][l], 64),
        "lb0": inp["hgrn_lb_logits"][0], "lb1": inp["hgrn_lb_logits"][1], "hng": inp["hgrn_norm_g"][l],
        "fcw0": inp["ffn_conv_w"][l][0], "fcw1": inp["ffn_conv_w"][l][1], "fcw2": inp["ffn_conv_w"][l][2],
        "fcb": inp["ffn_conv_b"][l], "fing": inp["final_norm_g"],
    }
    out = np.zeros((128, NVEC), np.float32)
    for n, ln in VEC_SPEC:
        a = np.asarray(v[n], np.float32).reshape(ln // 128, 128)
        out[:, VEC_OFF[n]:VEC_OFF[n] + ln // 128] = a.T
    return out


CST_SPEC = [("ident", 128), ("ones", 128), ("bd64", 128), ("rot", 128), ("m_gla", 256), ("m_rw", 512), ("m_rwn", 128), ("m_rwn4", 512),
            ("m_att", 512), ("opad", 256), ("cos", 2048), ("sin", 2048)]
CST_OFF = {}
_o = 0
for _n, _l in CST_SPEC:
    CST_OFF[_n] = _o
    _o += _l
NCST = _o


def _consts():
    c = np.zeros((128, NCST), np.float32)
    p = np.arange(128)[:, None]
    f = np.arange(128)[None, :]
    c[:, CST_OFF["ident"]:CST_OFF["ident"] + 128] = (p == f)
    c[:, CST_OFF["ones"]:CST_OFF["ones"] + 128] = 1.0
    c[:, CST_OFF["bd64"]:CST_OFF["bd64"] + 128] = (p // 64 == f // 64)
    rot = np.where((f % 64 < 32) & (p == f + 32), -1.0, 0.0) + np.where((f % 64 >= 32) & (p == f - 32), 1.0, 0.0)
    c[:, CST_OFF["rot"]:CST_OFF["rot"] + 128] = rot
    m = ((p // 64 == f // 64) & (p <= f)).astype(np.float32)
    c[:, CST_OFF["m_gla"]:CST_OFF["m_gla"] + 256] = np.tile(m, (1, 2))
    strict = (p < f).astype(np.float32)
    incl = (p <= f).astype(np.float32)
    c[:, CST_OFF["m_rw"]:CST_OFF["m_rw"] + 512] = np.concatenate([strict, incl, strict, incl], axis=1)
    c[:, CST_OFF["m_rwn"]:CST_OFF["m_rwn"] + 128] = (f < p)
    c[:, CST_OFF["m_rwn4"]:CST_OFF["m_rwn4"] + 512] = np.tile((f < p).astype(np.float32), (1, 4))
    cur = (p <= f).astype(np.float32)
    prv = (p >= f).astype(np.float32)
    c[:, CST_OFF["m_att"]:CST_OFF["m_att"] + 512] = np.concatenate([cur, prv, cur, prv], axis=1)
    op = np.zeros((128, 256), np.float32)
    op[:, 0:64] = 1.0
    op[:, 128 + 64:256] = 1.0
    c[:, CST_OFF["opad"]:CST_OFF["opad"] + 256] = op
    half = 32
    inv = 10000.0 ** (-np.arange(half, dtype=np.float32) / half)
    cidx = np.arange(128) % 64
    ang = np.arange(T, dtype=np.float32)[None, :] * inv[cidx % 32][:, None]
    c[:, CST_OFF["cos"]:CST_OFF["cos"] + T] = np.cos(ang)
    c[:, CST_OFF["sin"]:CST_OFF["sin"] + T] = np.sin(ang)
    return c


class Kern:
    def __init__(self, nc, P, dr, dbg=None):
        self.nc = nc
        self.P = P
        self.dr = dr
        self.dbg = dbg
        self.dbg_toks = []
        self.psi = 0
        self.nrot = 6
        self.wbi = 0
        sb = P.sb
        self.PS = [P.ps(f"ps{i}", [128, 512], F32) for i in range(8)]
        self.FS = [sb(f"fs{i}", [128, T], F32) for i in range(9)]
        self.HS = [sb(f"hs{i}", [128, T], BF16) for i in range(9)]
        self.xnT = sb("xnT", [128, 8, T], BF16)
        self.WB = [sb(f"wb{i}", [128, 1024], BF16) for i in range(4)]
        yab = sb("yAB", [128, 3 * T], BF16)
        yc = sb("yCt", [128, 2 * T], BF16)
        yd = sb("yDt", [128, 2 * T], BF16)
        self.yA = [TT(yab.t[:, i * T:(i + 1) * T], Buf(f"yA{i}")) for i in range(2)]
        self.yB = [TT(yab.t[:, 2 * T:3 * T], Buf("yB0"))]
        self.yC = [TT(yc.t[:, i * T:(i + 1) * T], Buf(f"yC{i}")) for i in range(2)]
        self.yD = [TT(yd.t[:, i * T:(i + 1) * T], Buf(f"yD{i}")) for i in range(2)]
        self.cg = TT(yab.t[:, 0:2 * T].bitcast(F32), [self.yA[0].b, self.yA[1].b])
        self.AR = TT(yc.t[:, :], [self.yC[0].b, self.yC[1].b])
        self.ydt = yd
        self.xu = [TT(yd.t[:, 3584 + j * 128:3584 + (j + 1) * 128], Buf(f"xu{j}")) for j in range(4)]
        self.nm_bufs = []
        self.hb = [Buf(f"hscr{i}") for i in range(8)]
        self.vec = [sb(f"vec{l}", [128, NVEC], F32) for l in range(DEPTH)]
        self.identF = sb("identF", [128, 128], F32)
        self.cst = {}
        for n, ln in CST_SPEC:
            if n in ("cos", "sin"):
                continue
            self.cst[n] = sb("c_" + n, [128, ln], BF16)
        self.small = sb("small", [128, 64], F32)
        self.tmpA = [sb(f"tmpA{i}", [128, 512], F32) for i in range(3)]
        self.tmpB = [sb(f"tmpB{i}", [128, 512], BF16) for i in range(3)]
        self.tai = 0
        self.tbi = 0
        self.Sb = TT(self.FS[8].t[:, :].bitcast(BF16).rearrange("p (n v) -> p n v", v=128), self.FS[8].b)
        self.pcx = sb("pcx", [128, 3 + T], F32)
        self.Sf = [sb(f"Sf{i}", [128, 128], F32) for i in range(2)]
        self.chs = sb("chs", [128, 4, 32], F32)
        self.misc = sb("miscw_sb", [128, 256], BF16)

    def nps(self):
        p = self.PS[self.psi]
        self.psi = (self.psi + 1) % self.nrot
        return p

    def ta(self):
        t = self.tmpA[self.tai]
        self.tai = (self.tai + 1) % 3
        return t

    def tb(self):
        t = self.tmpB[self.tbi]
        self.tbi = (self.tbi + 1) % 3
        return t

    def mm(self, out, lhsT, rhs, start, stop, reads, writes):
        self.P.op("pe", lambda e: e.matmul(out, lhsT=lhsT, rhs=rhs, start=start, stop=stop), reads, writes)

    def tr(self, out, in_, ident, reads, writes):
        self.P.op("pe", lambda e: e.transpose(out, in_, ident), reads, writes)

    def act(self, out, in_, func, reads, writes, scale=None, bias=None):
        kw = {}
        if scale is not None:
            kw["scale"] = scale
        if bias is not None:
            kw["bias"] = bias
        self.P.op("act", lambda e: e.activation(out=out, in_=in_, func=func, **kw), reads, writes)

    def tt(self, out, in0, in1, op, reads, writes, eng="dve"):
        self.P.op(eng, lambda e: e.tensor_tensor(out=out, in0=in0, in1=in1, op=op), reads, writes)

    def ts(self, out, in0, s1, s2, op0, op1, reads, writes, eng="dve"):
        if op1 is None:
            self.P.op(eng, lambda e: e.tensor_scalar(out=out, in0=in0, scalar1=s1, scalar2=None, op0=op0), reads, writes)
        else:
            self.P.op(eng, lambda e: e.tensor_scalar(out=out, in0=in0, scalar1=s1, scalar2=s2, op0=op0, op1=op1), reads, writes)

    def stt(self, out, in0, scalar, in1, op0, op1, reads, writes):
        self.P.op("dve", lambda e: e.scalar_tensor_tensor(out=out, in0=in0, scalar=scalar, in1=in1, op0=op0, op1=op1), reads, writes)

    def cp(self, out, in_, reads, writes, eng="act"):
        if eng == "act":
            self.P.op("act", lambda e: e.copy(out=out, in_=in_), reads, writes)
        else:
            self.P.op(eng, lambda e: e.tensor_copy(out=out, in_=in_), reads, writes)

    def memset(self, ap, val, writes, eng="dve"):
        self.P.op(eng, lambda e: e.memset(ap, val), (), writes)

    def vcol(self, l, name, j=0):
        o = VEC_OFF[name] + j
        return self.vec[l][:, o:o + 1]

    def dump(self, idx, tt_, ap):
        if self.dbg is None:
            return
        tok = self.P.dma("pool", self.dbg[idx], ap, reads=[tt_])
        self.dbg_toks.append(tok)

    def wload(self, src_ap, kc, m):
        wb = self.WB[self.wbi]
        self.wbi = (self.wbi + 1) % len(self.WB)
        view = wb.t[:, 0:kc * m].rearrange("p (k m) -> p k m", m=m)
        self.P.dma("pool", view, src_ap, writes=[wb])
        return wb, view

    def win_tile(self, l, name):
        c = WIN_TILES[name]
        src = self.dr["w_in"][l, :, c * 128:(c + 1) * 128].rearrange("(k p) m -> p k m", p=128)
        return self.wload(src, 8, 128)

    def projT(self, wb, wv, consume, rhs=None):
        for tb in range(4):
            ps = self.nps()
            for kc in range(8):
                self.mm(ps[:, :], wv[:, kc, :], self.xnT[:, kc, tb * 512:(tb + 1) * 512], kc == 0, kc == 7,
                        [wb, self.xnT], [ps])
            consume(tb, ps)

    def setup(self):
        P = self.P
        dr = self.dr
        P.dma("sp", self.identF[:, :], dr["cst"][:, CST_OFF["ident"]:CST_OFF["ident"] + 128], writes=[self.identF])
        for n, ln in CST_SPEC:
            if n in ("cos", "sin"):
                continue
            P.dma("pool", self.cst[n][:, :], dr["cst"][:, CST_OFF[n]:CST_OFF[n] + ln], writes=[self.cst[n]])
        for l in range(DEPTH):
            P.dma("sp", self.vec[l][:, :], dr["vecs"][l], writes=[self.vec[l]])
        self.memset(self.small[:, :], 0.0, [self.small])
        self.memset(self.small[:, 0:1], 1.0, [self.small])

    def load_x(self):
        P = self.P
        xin = [self.FS[8], None]
        for tt_ in range(NTT):
            half = tt_ % 2
            stg = self.FS[8].t[:, half * 1024:(half + 1) * 1024]
            if tt_ < 2:
                if tt_ == 0:
                    self._xb = [Buf("xin0"), Buf("xin1")]
            xb = self._xb[half]
            P.dma("sp", stg, self.dr["x"][tt_ * 128:(tt_ + 1) * 128, :], writes=[xb])
            for kc in range(8):
                ps = self.nps()
                self.tr(ps[:, 0:128], stg[:, kc * 128:(kc + 1) * 128], self.identF[:, :], [xb, self.identF], [ps])
                if kc % 2 == 0:
                    self.cp(self.FS[kc][:, tt_ * 128:(tt_ + 1) * 128], ps[:, 0:128], [ps], [self.FS[kc]])
                else:
                    self.cp(self.FS[kc][:, tt_ * 128:(tt_ + 1) * 128], ps[:, 0:128], [ps], [self.FS[kc]], eng="dve")
        self.FS[8].b.r.extend(self._xb[0].r + self._xb[1].r)
        if self._xb[0].w:
            self.FS[8].b.r.append(self._xb[0].w)
        if self._xb[1].w:
            self.FS[8].b.r.append(self._xb[1].w)

    def rmsnorm(self, l, gname, to_xn=True, out_fn=None):
        ones = self.cst["ones"]
        for tb in range(4):
            sl = slice(tb * 512, (tb + 1) * 512)
            ps = self.nps()
            for kc in range(8):
                sq = self.tb()
                self.act(sq[:, :], self.FS[kc][:, sl], AF.Square, [self.FS[kc]], [sq])
                self.mm(ps[:, :], ones[:, :], sq[:, :], kc == 0, kc == 7, [ones, sq], [ps])
            lnv = self.ta()
            self.act(lnv[:, :], ps[:, :], AF.Ln, [ps], [lnv], scale=1.0 / D, bias=self.small[:, 1:2])
            rstd = self.ta()
            self.act(rstd[:, :], lnv[:, :], AF.Exp, [lnv], [rstd], scale=-0.5)
            for kc in range(8):
                if to_xn:
                    self.stt(self.xnT[:, kc, sl], self.FS[kc][:, sl], self.vcol(l, gname, kc), rstd[:, :],
                             ALU.mult, ALU.mult, [self.FS[kc], rstd, self.vec[l]], [self.xnT])
                else:
                    out_fn(tb, kc, rstd)

    def gla(self, qF, kF, GF, Dt, Et, Hq, Hk, Hqh, Hkh, khtm, vsel, vrows, vdeps, DV, out_fn, DE2):
        P = self.P
        chs = self.chs
        onecol = self.small[:, 0:1]
        P.op("dve", lambda e: e.tensor_tensor_scan(out=GF[:, :], data0=onecol.to_broadcast([128, T]), data1=GF[:, :],
                                                   initial=0.0, op0=ALU.mult, op1=ALU.add), [GF, self.small], [GF])
        B3 = GF[:, :].rearrange("p (n l) -> p n l", l=64)
        D3 = Dt[:, :].rearrange("p (n l) -> p n l", l=64)
        self.memset(chs[:, 0, 0:1], 0.0, [chs])
        self.cp(chs[:, 0, 1:32], GF[:, 63:T - 64:64], [GF], [chs], eng="dve")
        self.cp(chs[:, 1, :], GF[:, 63:T:64], [GF], [chs], eng="dve")
        self.tt(chs[:, 3, :], chs[:, 1, :], chs[:, 0, :], ALU.subtract, [chs], [chs])
        self.act(chs[:, 2, :], chs[:, 3, :], AF.Exp, [chs], [chs])
        bmid = B3[:, :, 31:32].to_broadcast([128, 32, 64])
        bp = chs[:, 0, :].unsqueeze(2).to_broadcast([128, 32, 64])
        be = chs[:, 1, :].unsqueeze(2).to_broadcast([128, 32, 64])
        Dt2, Et2 = DE2
        D3b = Dt2[:, :].rearrange("p (n l) -> p n l", l=64)
        self.tt(D3, B3, bmid, ALU.subtract, [GF], [Dt])
        self.act(Et[:, :], Dt[:, :], AF.Exp, [Dt], [Et])
        self.act(Et2[:, :], Dt[:, :], AF.Exp, [Dt], [Et2], scale=-1.0)
        self.tt(D3b, B3, bp, ALU.subtract, [GF, chs], [Dt2])
        self.tt(Hq[:, :], qF[:, :], Et[:, :], ALU.mult, [qF, Et], [Hq])
        self.tt(Hk[:, :], kF[:, :], Et2[:, :], ALU.mult, [kF, Et2], [Hk])
        self.act(Et[:, :], Dt2[:, :], AF.Exp, [Dt2], [Et])
        self.tt(D3, be, B3, ALU.subtract, [GF, chs], [Dt])
        self.act(Et2[:, :], Dt[:, :], AF.Exp, [Dt], [Et2])
        self.tt(Hqh[:, :], qF[:, :], Et[:, :], ALU.mult, [qF, Et], [Hqh])
        self.tt(Hkh[:, :], kF[:, :], Et2[:, :], ALU.mult, [kF, Et2], [Hkh])
        self.to_tm(Hkh, khtm)
        kh3 = khtm[:, :].rearrange("p (t c) -> p t c", c=128)
        Sb = self.Sb
        self.memset(Sb[:, 0, :], 0.0, [Sb])
        self.memset(self.Sf[0][:, :], 0.0, [self.Sf[0]])
        per = 512 // DV
        psh = [None, None]
        for n in range(32):
            tt_, half = n // 2, n % 2
            if tt_ % per == 0:
                psh[half] = self.nps()
            ps = psh[half]
            c0 = (tt_ % per) * DV
            for hh in range(2):
                po = hh * 64
                self.mm(ps[po:po + 64, c0:c0 + DV], kh3[half * 64:half * 64 + 64, tt_, po:po + 64], vrows(tt_, half, hh),
                        True, True, [khtm] + vdeps, [ps])
            sfo, sfn = self.Sf[n % 2], self.Sf[(n + 1) % 2]
            self.stt(sfn[:, 0:DV], sfo[:, 0:DV], chs[:, 2, n:n + 1], ps[:, c0:c0 + DV], ALU.mult, ALU.add,
                     [sfo, chs, ps], [sfn])
            if n < 31:
                self.cp(Sb[:, n + 1, 0:DV], sfn[:, 0:DV], [sfn], [Sb])
        mg = self.cst["m_gla"]
        pso = [None, None]

        def scores(tt_):
            cs = slice(tt_ * 128, (tt_ + 1) * 128)
            at = self.tb()
            for hh in range(2):
                po = hh * 64
                pss = self.nps()
                self.mm(pss[:, 0:128], Hk[po:po + 64, cs], Hq[po:po + 64, cs], True, True, [Hk, Hq], [pss])
                self.tt(at[:, hh * 128:(hh + 1) * 128], pss[:, 0:128], mg[:, 0:128], ALU.mult, [pss, mg], [at])
            return at

        ats = {0: scores(0)}
        for tt_ in range(NTT):
            if tt_ + 1 < NTT:
                ats[tt_ + 1] = scores(tt_ + 1)
            at = ats.pop(tt_)
            if tt_ % 4 == 0:
                pso = [self.PS[6], self.PS[7]]
            c0 = (tt_ % 4) * 128
            for hh in range(2):
                po = hh * 64
                po_ = pso[hh]
                self.mm(po_[0:DV, c0:c0 + 128], vsel(tt_, hh), at[:, hh * 128:(hh + 1) * 128], True, False,
                        vdeps + [at], [po_])
                for half in range(2):
                    n = 2 * tt_ + half
                    self.mm(po_[0:DV, c0 + half * 64:c0 + half * 64 + 64], Sb[po:po + 64, n, 0:DV],
                            Hqh[po:po + 64, n * 64:(n + 1) * 64], False, half == 1, [Sb, Hqh], [po_])
            if tt_ % 4 == 3:
                for hh in range(2):
                    out_fn(hh, tt_ // 4, pso[hh])

    def to_tm(self, srcH, dst, dview=None):
        idb = self.cst["ident"]
        d3 = dst[:, :].rearrange("p (t c) -> p t c", c=128) if dview is None else dview
        for g4 in range(4):
            ps = self.nps()
            pb = ps.t[:, :].bitcast(BF16)
            for j in range(4):
                tt_ = g4 * 4 + j
                self.tr(pb[:, j * 128:(j + 1) * 128], srcH[:, tt_ * 128:(tt_ + 1) * 128], idb[:, :], [srcH, idb], [ps])
            self.cp(d3[:, g4 * 4:(g4 + 1) * 4, :], pb[:, 0:512].rearrange("p (t c) -> p t c", c=128), [ps], [dst])

    def hgrn2(self, l):
        FS, HS = self.FS, self.HS
        for i in range(2):
            qF, kF, GF, Dt, Et, oT = FS[0], FS[1], FS[2], FS[3], FS[4], FS[5]
            Hq, Hk, Hqh, Hkh, Hv, Hg, khtm, vtm = HS[0], HS[1], HS[2], HS[3], HS[4], HS[5], HS[6], HS[7]
            lbc = self.small[:, 4 + i:5 + i]
            omc = self.small[:, 6 + i:7 + i]
            wb, wv = self.win_tile(l, f"D_q{i}")
            self.projT(wb, wv, lambda tb, ps: self.act(qF[:, tb * 512:(tb + 1) * 512], ps[:, :], AF.Silu, [ps], [qF]))
            wb, wv = self.win_tile(l, f"D_f{i}")
            self.projT(wb, wv, lambda tb, ps: self.act(GF[:, tb * 512:(tb + 1) * 512], ps[:, :], AF.Sigmoid, [ps], [GF]))
            self.ts(GF[:, :], GF[:, :], omc, lbc, ALU.mult, ALU.add, [GF, self.small], [GF])
            self.ts(kF[:, :], GF[:, :], -1.0, 1.0, ALU.mult, ALU.add, [GF], [kF])
            self.act(GF[:, :], GF[:, :], AF.Ln, [GF], [GF])
            wb, wv = self.win_tile(l, f"D_i{i}")
            self.projT(wb, wv, lambda tb, ps: self.cp(Hv[:, tb * 512:(tb + 1) * 512], ps[:, :], [ps], [Hv]))
            wb, wv = self.win_tile(l, f"D_g{i}")
            self.projT(wb, wv, lambda tb, ps: self.act(Hg[:, tb * 512:(tb + 1) * 512], ps[:, :], AF.Sigmoid, [ps], [Hg]))
            self.to_tm(Hv, vtm)
            v3 = vtm[:, :].rearrange("p (t c) -> p t c", c=128)

            def out_fn(hh, g4, ps, oT=oT):
                self.cp(oT[hh * 64:hh * 64 + 64, g4 * 512:(g4 + 1) * 512], ps[0:64, :], [ps], [oT])

            self.gla(qF, kF, GF, Dt, Et, Hq, Hk, Hqh, Hkh, khtm,
                     lambda tt_, hh: v3[:, tt_, hh * 64:hh * 64 + 64],
                     lambda tt_, half, hh: v3[half * 64:half * 64 + 64, tt_, hh * 64:hh * 64 + 64],
                     [vtm], 64, out_fn, (FS[6], FS[7]))
            bd = self.cst["bd64"]
            for tb in range(4):
                sl = slice(tb * 512, (tb + 1) * 512)
                sq = self.tb()
                self.act(sq[:, :], oT[:, sl], AF.Square, [oT], [sq])
                ps = self.nps()
                self.mm(ps[:, :], bd[:, :], sq[:, :], True, True, [bd, sq], [ps])
                lnv = self.ta()
                self.act(lnv[:, :], ps[:, :], AF.Ln, [ps], [lnv], scale=1.0 / 64, bias=self.small[:, 1:2])
                rstd = self.ta()
                self.act(rstd[:, :], lnv[:, :], AF.Exp, [lnv], [rstd], scale=-0.5)
                self.tt(rstd[:, :], rstd[:, :], oT[:, sl], ALU.mult, [rstd, oT], [rstd])
                self.stt(self.yD[i][:, sl], rstd[:, :], self.vcol(l, "hng", i), Hg[:, sl], ALU.mult, ALU.mult,
                         [rstd, Hg, self.vec[l]], [self.yD[i]])

    def conv4(self, l, dst, stg, ci):
        w = lambda j: self.vcol(l, f"mcw{j}", ci)
        self.act(dst[:, :], stg[:, 0:T], AF.Identity, [stg, self.vec[l]], [dst], scale=w(3), bias=self.vcol(l, "mcb", ci))
        for j, sh in ((2, 1), (1, 2), (0, 3)):
            self.stt(dst[:, sh:T], stg[:, 0:T - sh], w(j), dst[:, sh:T], ALU.mult, ALU.add, [stg, dst, self.vec[l]], [dst])
        self.act(dst[:, :], dst[:, :], AF.Silu, [dst], [dst])

    def mlstm(self, l):
        FS, HS = self.FS, self.HS
        for i in range(2):
            qF, kF, GF, Dt, Et, hT, pcs, vaug_s = FS[0], FS[1], FS[2], FS[3], FS[4], FS[5], FS[6], FS[7]
            Hq, Hk, Hqh, Hkh, Hv, Hso, khtm = HS[0], HS[1], HS[2], HS[3], HS[4], HS[5], HS[6]
            stq = TT(self.pcx.t[:, 0:T], self.pcx.b)
            stk = pcs
            wb, wv = self.win_tile(l, f"C_q{i}")
            self.projT(wb, wv, lambda tb, ps: self.cp(stq[:, tb * 512:(tb + 1) * 512], ps[:, :], [ps], [stq]))
            wb2, wv2 = self.win_tile(l, f"C_k{i}")
            self.projT(wb2, wv2, lambda tb, ps: self.cp(stk[:, tb * 512:(tb + 1) * 512], ps[:, :], [ps], [stk]))
            self.conv4(l, qF, stq, i)
            self.conv4(l, kF, stk, 2 + i)
            wb, wv = self.win_tile(l, f"C_ig{i}")
            self.projT(wb, wv, lambda tb, ps: self.act(Et[:, tb * 512:(tb + 1) * 512], ps[:, :], AF.Exp, [ps, self.vec[l]], [Et],
                                                       bias=self.vcol(l, "i_b", i)))
            self.stt(kF[:, :], kF[:, :], 0.125, Et[:, :], ALU.mult, ALU.mult, [kF, Et], [kF])
            wb, wv = self.win_tile(l, f"C_fg{i}")
            nfb = self.small[:, 8 + i:9 + i]
            self.projT(wb, wv, lambda tb, ps: self.act(GF[:, tb * 512:(tb + 1) * 512], ps[:, :], AF.Exp, [ps, self.small], [GF],
                                                       scale=-1.0, bias=nfb))
            self.act(GF[:, :], GF[:, :], AF.Ln, [GF, self.small], [GF], bias=self.small[:, 0:1])
            self.ts(GF[:, :], GF[:, :], -1.0, None, ALU.mult, None, [GF], [GF])
            wb, wv = self.win_tile(l, f"C_v{i}")
            self.projT(wb, wv, lambda tb, ps: self.cp(Hv[:, tb * 512:(tb + 1) * 512], ps[:, :], [ps], [Hv]))
            wb, wv = self.win_tile(l, f"C_o{i}")
            self.projT(wb, wv, lambda tb, ps: self.act(Hso[:, tb * 512:(tb + 1) * 512], ps[:, :], AF.Sigmoid, [ps], [Hso]))
            va = vaug_s.t[:, :].bitcast(BF16).rearrange("p (t h c) -> p t h c", h=2, c=128)
            self.memset(va[:, :, :, 64:128], 1.0, [vaug_s])
            idb = self.cst["ident"]
            for g4 in range(4):
                ps = self.nps()
                pb = ps.t[:, :].bitcast(BF16)
                for j in range(4):
                    tt_ = g4 * 4 + j
                    self.tr(pb[:, j * 128:(j + 1) * 128], Hv[:, tt_ * 128:(tt_ + 1) * 128], idb[:, :], [Hv, idb], [ps])
                self.cp(va[:, g4 * 4:(g4 + 1) * 4, :, 0:64],
                        pb[:, 0:512].rearrange("p (t h c) -> p t h c", h=2, c=64), [ps], [vaug_s])

            def out_fn(hh, g4, ps, hT=hT):
                sl = slice(g4 * 512, (g4 + 1) * 512)
                den = self.ta()
                self.act(den[0:64, :], ps[64:128, :], AF.Abs, [ps], [den])
                self.ts(den[0:64, :], den[0:64, :], 1.0, None, ALU.max, None, [den], [den])
                self.act(den[0:64, :], den[0:64, :], AF.Ln, [den], [den])
                self.act(den[0:64, :], den[0:64, :], AF.Exp, [den], [den], scale=-1.0)
                self.tt(den[0:64, :], ps[0:64, :], den[0:64, :], ALU.mult, [ps, den], [den])
                self.cp(hT[hh * 64:hh * 64 + 64, sl], den[0:64, :], [den], [hT])

            self.gla(qF, kF, GF, Dt, Et, Hq, Hk, Hqh, Hkh, khtm,
                     lambda tt_, hh: va[:, tt_, hh, :],
                     lambda tt_, half, hh: va[half * 64:half * 64 + 64, tt_, hh, :],
                     [vaug_s], 128, out_fn, (FS[6], FS[8]))
            self.tt(self.yC[i][:, :], hT[:, :], Hso[:, :], ALU.mult, [hT, Hso], [self.yC[i]])

    def attn(self, l):
        FS, HS = self.FS, self.HS
        cos, sin = HS[3], HS[4]
        for nm_, tt_ in (("cos", cos), ("sin", sin)):
            self.P.dma("pool", tt_[:, :], self.dr["cst"][:, CST_OFF[nm_]:CST_OFF[nm_] + T], writes=[tt_])
        num, den = FS[0], FS[1]
        zr, zp = FS[2], FS[3]
        mk = self.cst["m_att"]
        opad = self.cst["opad"]
        op3 = opad[:, :].rearrange("p (j c) -> p j c", c=128)
        for g in range(3):
            d = (1, 4, 16)[g]
            Hq, Hk, Hv = HS[0], HS[1], HS[2]
            vpad_s = FS[4]
            rot = self.cst["rot"]
            qb = HS[5]
            for nm, dstH in (("q", Hq), ("k", Hk)):
                wb, wv = self.win_tile(l, f"B_{nm}{g}")

                def ev(tb, ps):
                    self.cp(zr[:, tb * 512:(tb + 1) * 512], ps[:, :], [ps], [zr])
                    self.cp(qb[:, tb * 512:(tb + 1) * 512], ps[:, :], [ps], [qb], eng="dve")

                self.projT(wb, wv, ev)
                for tb in range(4):
                    sl = slice(tb * 512, (tb + 1) * 512)
                    ps = self.nps()
                    self.mm(ps[:, :], rot[:, :], qb[:, sl], True, True, [rot, qb], [ps])
                    self.tt(zp[:, sl], ps[:, :], sin[:, sl], ALU.mult, [ps, sin], [zp])
                self.tt(zr[:, :], zr[:, :], cos[:, :], ALU.mult, [zr, cos], [zr])
                self.tt(dstH[:, :], zr[:, :], zp[:, :], ALU.add, [zr, zp], [dstH])
            wb, wv = self.win_tile(l, f"B_v{g}")
            self.projT(wb, wv, lambda tb, ps: self.cp(Hv[:, tb * 512:(tb + 1) * 512], ps[:, :], [ps], [Hv]))
            vp = vpad_s.t[:, :].bitcast(BF16).rearrange("p (b j c) -> p b j c", j=2, c=128)
            if g == 0:
                self.memset(vp[:, :, 0, 64:128], 0.0, [vpad_s])
                self.memset(vp[:, :, 1, 0:64], 0.0, [vpad_s])
            nb = 16 // d
            idb = self.cst["ident"]

            def cols(r, nbk):
                st = nbk * 128 * d + r
                return slice(st, st + 127 * d + 1, d)

            blks = [(r, nbk) for r in range(d) for nbk in range(nb)]
            for g4 in range(4):
                ps = self.nps()
                pb = ps.t[:, :].bitcast(BF16)
                for j in range(4):
                    r, nbk = blks[g4 * 4 + j]
                    self.tr(pb[:, j * 128:(j + 1) * 128], Hv[:, cols(r, nbk)], idb[:, :], [Hv, idb], [ps])
                src = pb[:, 0:512].rearrange("p (b j c) -> p b j c", j=2, c=64)
                self.cp(vp[:, g4 * 4:(g4 + 1) * 4, 0, 0:64], src[:, :, 0, :], [ps], [vpad_s])
                self.cp(vp[:, g4 * 4:(g4 + 1) * 4, 1, 64:128], src[:, :, 1, :], [ps], [vpad_s], eng="dve")
            def scores(bi):
                r, nbk = blks[bi]
                hasprev = nbk > 0
                pt = self.tb()
                for hh in range(2):
                    po = hh * 64
                    pss = self.nps()
                    wdt = 256 if hasprev else 128
                    self.mm(pss[:, 0:128], Hk[po:po + 64, cols(r, nbk)],
                            Hq[po:po + 64, cols(r, nbk)], True, True, [Hk, Hq], [pss])
                    if hasprev:
                        self.mm(pss[:, 128:256], Hk[po:po + 64, cols(r, nbk - 1)],
                                Hq[po:po + 64, cols(r, nbk)], True, True, [Hk, Hq], [pss])
                    c1 = hh * 256
                    self.act(pt[:, c1:c1 + wdt], pss[:, 0:wdt], AF.Exp, [pss], [pt], scale=0.125)
                    self.tt(pt[:, c1:c1 + wdt], pt[:, c1:c1 + wdt], mk[:, 0:wdt], ALU.mult, [pt, mk], [pt])
                return pt

            pts = {0: scores(0)}
            for b4 in range(4):
                psn, psd = self.PS[6], self.PS[7]
                for jb in range(4):
                    bi = b4 * 4 + jb
                    r, nbk = blks[bi]
                    hasprev = nbk > 0
                    if bi + 1 < 16:
                        pts[bi + 1] = scores(bi + 1)
                    pt = pts.pop(bi)
                    c0 = jb * 128
                    seq = []
                    for hh in range(2):
                        seq.append((hh, bi, (2 * hh) * 128))
                        if hasprev:
                            seq.append((hh, bi - 1, (2 * hh + 1) * 128))
                    for ii, (hh, vb, pc0) in enumerate(seq):
                        self.mm(psn[:, c0:c0 + 128], vp[:, vb, hh, :], pt[:, pc0:pc0 + 128], ii == 0, ii == len(seq) - 1,
                                [vpad_s, pt], [psn])
                    for ii, (hh, vb, pc0) in enumerate(seq):
                        self.mm(psd[:, c0:c0 + 128], op3[:, hh, :], pt[:, pc0:pc0 + 128], ii == 0, ii == len(seq) - 1,
                                [opad, pt], [psd])
                if d == 1:
                    dn = num[:, b4 * 512:(b4 + 1) * 512]
                    dd = den[:, b4 * 512:(b4 + 1) * 512]
                    sn, sd = psn[:, :], psd[:, :]
                elif d == 4:
                    dn = num[:, :].rearrange("p (n r) -> p r n", r=4)[:, b4, :]
                    dd = den[:, :].rearrange("p (n r) -> p r n", r=4)[:, b4, :]
                    sn, sd = psn[:, :], psd[:, :]
                else:
                    dn = num[:, :].rearrange("p (n r) -> p r n", r=16)[:, b4 * 4:(b4 + 1) * 4, :]
                    dd = den[:, :].rearrange("p (n r) -> p r n", r=16)[:, b4 * 4:(b4 + 1) * 4, :]
                    sn = psn[:, :].rearrange("p (r n) -> p r n", n=128)
                    sd = psd[:, :].rearrange("p (r n) -> p r n", n=128)
                if g == 0:
                    self.cp(dn, sn, [psn], [num])
                    self.cp(dd, sd, [psd], [den], eng="dve")
                else:
                    self.tt(dn, sn, dn, ALU.add, [psn, num], [num])
                    self.tt(dd, sd, dd, ALU.add, [psd, den], [den])
        self.act(den[:, :], den[:, :], AF.Ln, [den], [den])
        self.act(den[:, :], den[:, :], AF.Exp, [den], [den], scale=-1.0)
        self.tt(self.yB[0][:, :], num[:, :], den[:, :], ALU.mult, [num, den], [self.yB[0]])

    def rwkv(self, l):
        FS, HS = self.FS, self.HS
        P = self.P
        zm = FS[8]
        pcx = self.pcx
        self.memset(pcx[:, 2:3], 0.0, [pcx])

        def proj_lerp(name, dst, mucol):
            wb, wv = self.win_tile(l, name)
            self.projT(wb, wv, lambda tb, ps: self.cp(pcx[:, 3 + tb * 512:3 + (tb + 1) * 512], ps[:, :], [ps], [pcx]))
            self.tt(dst[:, :], pcx[:, 2:2 + T], pcx[:, 3:3 + T], ALU.subtract, [pcx], [dst])
            self.stt(dst[:, :], dst[:, :], mucol, pcx[:, 3:3 + T], ALU.mult, ALU.add, [dst, pcx, self.vec[l]], [dst])

        proj_lerp("A_m", zm, self.vcol(l, "mu_m", 0))
        zmb = HS[8]
        self.act(zmb[0:32, :], zm[0:32, :], AF.Tanh, [zm], [zmb])
        self.cp(zmb[32:64, :], zm[32:64, :], [zm], [zmb])
        self.act(zmb[64:128, :], zm[64:128, :], AF.Sigmoid, [zm], [zmb])
        misc = self.misc
        bd = self.cst["bd64"]
        mrw = self.cst["m_rw"]
        mrwn = self.cst["m_rwn"]
        idb = self.cst["ident"]
        for i in range(2):
            rF, kF, aF, kkF, BF_, B1, Dt, Et = FS[0], FS[1], FS[2], FS[3], FS[4], FS[5], FS[6], FS[7]
            Hv, Hr, Ha, Hb, Hk, Hr0, Ha0, Hx = HS[0], HS[1], HS[2], HS[3], HS[4], HS[5], HS[6], HS[7]
            proj_lerp(f"A_r{i}", rF, self.vcol(l, "mu_r", i))
            proj_lerp(f"A_k{i}", kF, self.vcol(l, "mu_k", i))
            proj_lerp(f"A_v{i}", Dt, self.vcol(l, "mu_v", i))
            self.cp(Hv[:, :], Dt[:, :], [Dt], [Hv])
            cs = slice(i * 128, (i + 1) * 128)
            for tb in range(4):
                sl = slice(tb * 512, (tb + 1) * 512)
                ps = self.nps()
                self.mm(ps[:, :], misc[0:32, cs], zmb[0:32, sl], True, True, [misc, zmb], [ps])
                self.act(BF_[:, sl], ps[:, :], AF.Sigmoid, [ps, self.vec[l]], [BF_], bias=self.vcol(l, "w0", i))
                ps = self.nps()
                self.mm(ps[:, :], misc[32:64, cs], zmb[32:64, sl], True, True, [misc, zmb], [ps])
                self.act(aF[:, sl], ps[:, :], AF.Sigmoid, [ps, self.vec[l]], [aF], bias=self.vcol(l, "a0", i))
            self.ts(BF_[:, :], BF_[:, :], -0.6065306597126334, None, ALU.mult, None, [BF_], [BF_])
            self.ts(kkF[:, :], kF[:, :], self.vcol(l, "k_k", i), None, ALU.mult, None, [kF, self.vec[l]], [kkF])
            for tb in range(4):
                sl = slice(tb * 512, (tb + 1) * 512)
                sq = self.tb()
                self.act(sq[:, :], kkF[:, sl], AF.Square, [kkF], [sq])
                ps = self.nps()
                self.mm(ps[:, :], bd[:, :], sq[:, :], True, True, [bd, sq], [ps])
                lnv = self.ta()
                self.act(lnv[:, :], ps[:, :], AF.Ln, [ps, self.small], [lnv], bias=self.small[:, 2:3])
                self.act(lnv[:, :], lnv[:, :], AF.Exp, [lnv], [lnv], scale=-0.5)
                self.tt(kkF[:, sl], kkF[:, sl], lnv[:, :], ALU.mult, [kkF, lnv], [kkF])
            self.ts(Dt[:, :], aF[:, :], -1.0, self.vcol(l, "k_a", i), ALU.add, ALU.mult, [aF, self.vec[l]], [Dt])
            self.stt(kF[:, :], Dt[:, :], 1.0, kF[:, :], ALU.add, ALU.mult, [Dt, kF], [kF])
            self.stt(Hx[:, :], rF[:, :], self.vcol(l, "r_k", i), kF[:, :], ALU.mult, ALU.mult, [rF, kF, self.vec[l]], [Hx])
            self.tt(aF[:, :], aF[:, :], kkF[:, :], ALU.mult, [aF, kkF], [aF])
            self.ts(kkF[:, :], kkF[:, :], -1.0, None, ALU.mult, None, [kkF], [kkF])
            onecol = self.small[:, 0:1]
            P.op("dve", lambda e: e.tensor_tensor_scan(out=B1[:, :], data0=onecol.to_broadcast([128, T]), data1=BF_[:, :],
                                                       initial=0.0, op0=ALU.mult, op1=ALU.add), [BF_, self.small], [B1])
            self.tt(BF_[:, :], B1[:, :], BF_[:, :], ALU.subtract, [B1, BF_], [BF_])
            Bc, Bm = B1, BF_
            chs = self.chs
            self.memset(chs[:, 0, 0:1], 0.0, [chs])
            self.cp(chs[:, 0, 1:16], Bc[:, 127:T - 128:128], [Bc], [chs], eng="dve")
            self.cp(chs[:, 1, 0:16], Bc[:, 127:T:128], [Bc], [chs], eng="dve")
            self.tt(chs[:, 3, 0:16], chs[:, 1, 0:16], chs[:, 0, 0:16], ALU.subtract, [chs], [chs])
            self.act(chs[:, 2, 0:16], chs[:, 3, 0:16], AF.Exp, [chs], [chs])
            B3 = Bc[:, :].rearrange("p (n l) -> p n l", l=128)
            M3 = Bm[:, :].rearrange("p (n l) -> p n l", l=128)
            D3 = Dt[:, :].rearrange("p (n l) -> p n l", l=128)
            bmid = B3[:, :, 63:64].to_broadcast([128, 16, 128])
            bp = chs[:, 0, 0:16].unsqueeze(2).to_broadcast([128, 16, 128])
            be = chs[:, 1, 0:16].unsqueeze(2).to_broadcast([128, 16, 128])
            AR = self.AR
            AR4 = AR[:, :].rearrange("p (n j t) -> p n j t", j=2, t=128)

            def expmul(dst_ap, srcF, X3, ref, sign, reads, writes):
                if sign > 0:
                    self.tt(D3, X3, ref, ALU.subtract, reads, [Dt])
                else:
                    self.tt(D3, ref, X3, ALU.subtract, reads, [Dt])
                self.act(Et[:, :], Dt[:, :], AF.Exp, [Dt], [Et])
                self.tt(dst_ap, srcF, Et[:, :].rearrange("p (n t) -> p n t", t=128), ALU.mult, [Et] + reads, writes)

            r3 = rF[:, :].rearrange("p (n t) -> p n t", t=128)
            na3 = kkF[:, :].rearrange("p (n t) -> p n t", t=128)
            b3 = aF[:, :].rearrange("p (n t) -> p n t", t=128)
            k3 = kF[:, :].rearrange("p (n t) -> p n t", t=128)
            h3 = lambda H: H[:, :].rearrange("p (n t) -> p n t", t=128)
            expmul(AR4[:, :, 1, :], r3, B3, bmid, +1, [Bc, rF], [AR])
            expmul(AR4[:, :, 0, :], na3, M3, bmid, +1, [Bm, Bc, kkF], [AR])
            self.tt(D3, bmid, B3, ALU.subtract, [Bc], [Dt])
            self.act(Et[:, :], Dt[:, :], AF.Exp, [Dt], [Et])
            self.tt(Hb[:, :], aF[:, :], Et[:, :], ALU.mult, [aF, Et], [Hb])
            self.tt(Hk[:, :], kF[:, :], Et[:, :], ALU.mult, [kF, Et], [Hk])
            expmul(h3(Hr0), r3, B3, bp, +1, [Bc, chs, rF], [Hr0])
            expmul(h3(Ha0), na3, M3, bp, +1, [Bm, chs, kkF], [Ha0])
            self.tt(D3, be, B3, ALU.subtract, [Bc, chs], [Dt])
            self.act(Et[:, :], Dt[:, :], AF.Exp, [Dt], [Et])
            self.tt(Hr[:, :], aF[:, :], Et[:, :], ALU.mult, [aF, Et], [Hr])
            self.tt(Ha[:, :], kF[:, :], Et[:, :], ALU.mult, [kF, Et], [Ha])
            bhtm, khtm, vtm = FS[0], FS[1], FS[2]
            tmv = lambda s: s.t[:, 0:1024].bitcast(BF16).rearrange("p (t c) -> p t c", c=128)
            self.to_tm(Hr, bhtm, tmv(bhtm))
            self.to_tm(Ha, khtm, tmv(khtm))
            self.to_tm(Hv, vtm, tmv(vtm))
            bh3, kh3, v3 = tmv(bhtm), tmv(khtm), tmv(vtm)
            yT = FS[3]
            Sb = self.Sb
            self.memset(Sb[:, 0, 0:64], 0.0, [Sb])
            self.memset(self.Sf[0][:, 0:64], 0.0, [self.Sf[0]])

            def sub_bufs(parent, n, nm_):
                out = []
                for j in range(n):
                    b = Buf(f"{nm_}{j}")
                    b.r = list(parent.b.r)
                    if parent.b.w is not None:
                        b.r.append(parent.b.w)
                    out.append(b)
                return out

            def merge_back(parent, bufs):
                for b in bufs:
                    parent.b.r.extend(b.r)
                    if b.w is not None:
                        parent.b.r.append(b.w)

            ATst, PTst, stparents = [], [], []
            for q in range(4):
                par = FS[4 + q]
                bl = sub_bufs(par, 8, f"ATst{q}_")
                stparents.append((par, bl))
                v = par.t[:, :].bitcast(BF16)
                for j in range(8):
                    ATst.append(TT(v[:, j * 512:(j + 1) * 512], bl[j]))
            PTg = []
            for q in range(2):
                par = FS[q]
                bl = sub_bufs(par, 4, f"PTst{q}_")
                stparents.append((par, bl))
                v = par.t[:, 1024:2048].bitcast(BF16)
                for j in range(16):
                    PTst.append(TT(v[:, j * 128:(j + 1) * 128], bl[j // 4]))
                for j in range(4):
                    PTg.append(TT(v[:, j * 512:(j + 1) * 512], bl[j]))
            self.nrot = 8
            ydt = self.ydt
            NN = [[TT(ydt.t[:, par * 1024 + h * 512: par * 1024 + (h + 1) * 512], Buf(f"NN{par}{h}")) for h in range(2)]
                  for par in range(2)]
            PTp = [[TT(ydt.t[:, 2048 + par * 512 + h * 256: 2048 + par * 512 + (h + 1) * 256], Buf(f"PTp{par}{h}"))
                    for h in range(2)] for par in range(2)]
            N0t = TT(ydt.t[:, 3072:3584], Buf("N0t"))
            mrwn4 = self.cst["m_rwn4"]
            self.nm_bufs.extend(NN[0] + NN[1] + PTp[0] + PTp[1] + [N0t])
            def score_stage(g8):
                items = [(2 * g8 + j // 2, j % 2) for j in range(4)]
                ps2h = [self.nps(), self.nps()]
                for s_, (n, hh) in enumerate(items):
                    po = hh * 64
                    cs_ = slice(n * 128, (n + 1) * 128)
                    it = n * 2 + hh
                    AT = ATst[it]
                    ps = self.nps()
                    arn = AR4[po:po + 64, n, :, :].rearrange("p j t -> p (j t)")
                    self.mm(ps[:, 0:256], Hb[po:po + 64, cs_], arn, True, True, [Hb, AR], [ps])
                    self.mm(ps[:, 256:512], Hk[po:po + 64, cs_], arn, True, True, [Hk, AR], [ps])
                    self.tt(AT[:, :], ps[:, :], mrw[:, :], ALU.mult, [ps, mrw], [AT])
                    self.mm(ps2h[hh][:, (s_ // 2) * 128:(s_ // 2 + 1) * 128], AR4[po:po + 64, n, 0, :], Hb[po:po + 64, cs_],
                            True, True, [AR, Hb], [ps2h[hh]])
                n0v = N0t[:, :].rearrange("p (j h x) -> p j h x", h=2, x=128)
                for hh in range(2):
                    self.tt(n0v[:, :, hh, :], ps2h[hh][:, 0:256].rearrange("p (j x) -> p j x", x=128),
                            mrwn4[:, 0:256].rearrange("p (j x) -> p j x", x=128), ALU.mult, [ps2h[hh], mrwn4], [N0t])

            score_stage(0)
            for g8 in range(8):
                items = [(2 * g8 + j // 2, j % 2) for j in range(4)]
                for s_, (n, hh) in enumerate(items):
                    AT = ATst[n * 2 + hh]
                    p0 = PTp[0][s_ // 2]
                    self.tt(p0[:, (s_ % 2) * 128:(s_ % 2 + 1) * 128], AT[:, 0:128], idb[:, :], ALU.add, [AT, idb], [p0])
                for lev in range(6):
                    par = lev % 2
                    for h in range(2):
                        psa = self.nps()
                        for q2 in range(2):
                            s_ = 2 * h + q2
                            n, hh = items[s_]
                            if lev == 0:
                                cN, cNT = N0t[:, s_ * 128:(s_ + 1) * 128], ATst[n * 2 + hh][:, 0:128]
                                rd = [N0t, ATst[n * 2 + hh]]
                            else:
                                src = NN[1 - par][h]
                                cN, cNT = src[:, q2 * 256:q2 * 256 + 128], src[:, q2 * 256 + 128:q2 * 256 + 256]
                                rd = [src]
                            self.mm(psa[:, q2 * 256:q2 * 256 + 128], cNT, cN, True, True, rd, [psa])
                            if lev < 5:
                                self.mm(psa[:, q2 * 256 + 128:q2 * 256 + 256], cN, cNT, True, True, rd, [psa])
                        dst = NN[par][h]
                        if lev < 5:
                            self.cp(dst[:, :], psa[:, :], [psa], [dst])
                        else:
                            self.cp(dst[:, :].rearrange("p (q x) -> p q x", x=256)[:, :, 0:128],
                                    psa[:, :].rearrange("p (q x) -> p q x", x=256)[:, :, 0:128], [psa], [dst])
                    for h in range(2):
                        psc = self.nps()
                        pcur = PTp[par][h]
                        for q2 in range(2):
                            nN = NN[par][h][:, q2 * 256:q2 * 256 + 128]
                            self.mm(psc[:, q2 * 128:(q2 + 1) * 128], nN, pcur[:, q2 * 128:(q2 + 1) * 128], True, True,
                                    [NN[par][h], pcur], [psc])
                        if lev == 5:
                            PT2 = TT(PTg[g8].t[:, h * 256:(h + 1) * 256], PTg[g8].b)
                        else:
                            PT2 = PTp[1 - par][h]
                        self.tt(PT2[:, :], psc[:, 0:256], pcur[:, :], ALU.add, [psc, pcur], [PT2])
                    if lev == 0 and g8 + 1 < 8:
                        score_stage(g8 + 1)
            self.nrot = 6
            self.psi = self.psi % 6
            for n in range(16):
                cs_ = slice(n * 128, (n + 1) * 128)
                Us = []
                psxs = []
                for hh in range(2):
                    po = hh * 64
                    AT = ATst[n * 2 + hh]
                    psx = self.nps()
                    self.mm(psx[:, 0:64], AT[:, 256:384], v3[:, n, po:po + 64], True, False, [AT, vtm], [psx])
                    self.mm(psx[:, 0:64], Ha0[po:po + 64, cs_], Sb[po:po + 64, n, 0:64], False, True, [Ha0, Sb], [psx])
                    psxs.append(psx)
                for hh in range(2):
                    X = self.xu[2 * hh]
                    self.cp(X[:, 0:64], psxs[hh][:, 0:64], [psxs[hh]], [X], eng=("act" if hh == 0 else "dve"))
                psus = []
                for hh in range(2):
                    PT = PTst[n * 2 + hh]
                    X = self.xu[2 * hh]
                    psu = self.nps()
                    self.mm(psu[:, 0:64], PT[:, 0:128], X[:, 0:64], True, True, [PT, X], [psu])
                    psus.append(psu)
                for hh in range(2):
                    U = self.xu[2 * hh + 1]
                    self.cp(U[:, 0:64], psus[hh][:, 0:64], [psus[hh]], [U], eng=("dve" if hh == 0 else "act"))
                    Us.append(U)
                psS = self.nps()
                for hh in range(2):
                    po = hh * 64
                    U = Us[hh]
                    self.mm(psS[po:po + 64, 0:64], bh3[:, n, po:po + 64], U[:, 0:64], True, False, [bhtm, U], [psS])
                    self.mm(psS[po:po + 64, 0:64], kh3[:, n, po:po + 64], v3[:, n, po:po + 64], False, True, [khtm, vtm], [psS])
                sfo, sfn = self.Sf[n % 2], self.Sf[(n + 1) % 2]
                self.stt(sfn[:, 0:64], sfo[:, 0:64], chs[:, 2, n:n + 1], psS[:, 0:64], ALU.mult, ALU.add, [sfo, chs, psS], [sfn])
                self.cp(Sb[:, n + 1, 0:64], sfn[:, 0:64], [sfn], [Sb])
                for hh in range(2):
                    po = hh * 64
                    AT, U = ATst[n * 2 + hh], Us[hh]
                    psy = self.nps()
                    self.mm(psy[0:64, 0:128], v3[:, n, po:po + 64], AT[:, 384:512], True, False, [vtm, AT], [psy])
                    self.mm(psy[0:64, 0:128], U[:, 0:64], AT[:, 128:256], False, False, [U, AT], [psy])
                    self.mm(psy[0:64, 0:128], Sb[po:po + 64, n, 0:64], Hr0[po:po + 64, cs_], False, True, [Sb, Hr0], [psy])
                    self.cp(yT[po:po + 64, cs_], psy[0:64, 0:128], [psy], [yT])
            for par, bl in stparents:
                merge_back(par, bl)
            for tb in range(4):
                sl = slice(tb * 512, (tb + 1) * 512)
                yb = self.tb()
                self.cp(yb[:, :], yT[:, sl], [yT], [yb])
                ps = self.nps()
                self.mm(ps[:, :], bd[:, :], yb[:, :], True, True, [bd, yb], [ps])
                yc = self.ta()
                self.stt(yc[:, :], ps[:, :], -1.0 / 64, yT[:, sl], ALU.mult, ALU.add, [ps, yT], [yc])
                sq = self.tb()
                self.act(sq[:, :], yc[:, :], AF.Square, [yc], [sq])
                ps2 = self.nps()
                self.mm(ps2[:, :], bd[:, :], sq[:, :], True, True, [bd, sq], [ps2])
                lnv = self.ta()
                self.act(lnv[:, :], ps2[:, :], AF.Ln, [ps2, self.small], [lnv], scale=1.0 / 64, bias=self.small[:, 3:4])
                self.act(lnv[:, :], lnv[:, :], AF.Exp, [lnv], [lnv], scale=-0.5)
                self.tt(yc[:, :], yc[:, :], lnv[:, :], ALU.mult, [yc, lnv], [yc])
                self.ts(yc[:, :], yc[:, :], self.vcol(l, "ln_g", i), self.vcol(l, "ln_b", i), ALU.mult, ALU.add,
                        [yc, self.vec[l]], [yc])
                ps3 = self.nps()
                self.mm(ps3[:, :], bd[:, :], Hx[:, sl], True, True, [bd, Hx], [ps3])
                bo = self.ta()
                self.tt(bo[:, :], ps3[:, :], Hv[:, sl], ALU.mult, [ps3, Hv], [bo])
                self.tt(yc[:, :], yc[:, :], bo[:, :], ALU.add, [yc, bo], [yc])
                ps4 = self.nps()
                self.mm(ps4[:, :], misc[64:128, cs], zmb[64:128, sl], True, True, [misc, zmb], [ps4])
                self.tt(self.yA[i][:, sl], ps4[:, :], yc[:, :], ALU.mult, [ps4, yc], [self.yA[i]])
        for t_ in self.nm_bufs + self.xu:
            for yb_ in self.yD:
                yb_.b.r.extend(t_.b.r)
                if t_.b.w is not None:
                    yb_.b.r.append(t_.b.w)
        self.nm_bufs = []

    def merge_wout(self, l):
        P = self.P
        ys = [(self.yA, 0, 2), (self.yB, 2, 1), (self.yC, 3, 2), (self.yD, 5, 2)]
        mg = self.HS[0:8]
        acc = self.FS[8]
        for dt in range(8):
            for b in range(4):
                wb, wv = self.win_tile(l, f"G_{b}_{dt}")
                yl, k0, nk = ys[b]
                psrc = self.dr["pall"][l, k0 * 128:(k0 + nk) * 128, dt * 128:(dt + 1) * 128].rearrange("(k p) m -> p k m", p=128)
                pwb, pwv = self.wload(psrc, nk, 128)
                for tb in range(4):
                    sl = slice(tb * 512, (tb + 1) * 512)
                    ps = self.nps()
                    for kc in range(8):
                        self.mm(ps[:, :], wv[:, kc, :], self.xnT[:, kc, sl], kc == 0, kc == 7, [wb, self.xnT], [ps])
                    gs = self.ta()
                    self.act(gs[:, :], ps[:, :], AF.Sigmoid, [ps, self.vec[l]], [gs], bias=self.vcol(l, "bgate", b * 8 + dt))
                    pp = self.nps()
                    for kc in range(nk):
                        self.mm(pp[:, :], pwv[:, kc, :], yl[kc][:, sl], kc == 0, kc == nk - 1,
                                [pwb, yl[kc]], [pp])
                    if b == 0:
                        self.tt(acc[:, sl], pp[:, :], gs[:, :], ALU.mult, [pp, gs], [acc])
                    else:
                        self.tt(gs[:, :], pp[:, :], gs[:, :], ALU.mult, [pp, gs], [gs])
                        if b < 3:
                            self.tt(acc[:, sl], acc[:, sl], gs[:, :], ALU.add, [acc, gs], [acc])
                        else:
                            self.tt(mg[dt][:, sl], acc[:, sl], gs[:, :], ALU.add, [acc, gs], [mg[dt]])
        for kc in range(8):
            P.dma("sp", self.FS[kc][:, :], self.dr["hscr"][kc], reads=[self.hb[kc]], writes=[self.FS[kc]])
        for dt in range(8):
            src = self.dr["w_out"][l, :, dt * 128:(dt + 1) * 128].rearrange("(k p) m -> p k m", p=128)
            wb, wv = self.wload(src, 8, 128)
            for tb in range(4):
                sl = slice(tb * 512, (tb + 1) * 512)
                ps = self.nps()
                for kc in range(8):
                    self.mm(ps[:, :], wv[:, kc, :], mg[kc][:, sl], kc == 0, kc == 7, [wb, mg[kc]], [ps])
                self.tt(self.FS[dt][:, sl], ps[:, :], self.FS[dt][:, sl], ALU.add, [ps, self.FS[dt]], [self.FS[dt]])

    def ffn(self, l):
        FS, HS = self.FS, self.HS
        groups = [list(range(0, 6)), list(range(6, 12)), list(range(12, 17)), list(range(17, 22))]
        stgs = {"u": TT(self.pcx.t[:, 0:T], self.pcx.b), "g": TT(self.AR.t[:, :].bitcast(F32), self.AR.b)}
        cu, cg = FS[8], self.cg

        def up_tile(j):
            for which, dstF, cidx in (("u", cu, j), ("g", cg, NFT + j)):
                stg = stgs[which]
                col0 = (0 if which == "u" else DFF) + j * 128
                src = self.dr["w_up"][l, :, col0:col0 + 128].rearrange("(k p) m -> p k m", p=128)
                wb, wv = self.wload(src, 8, 128)
                self.projT(wb, wv, lambda tb, ps, stg=stg: self.cp(stg[:, tb * 512:(tb + 1) * 512], ps[:, :], [ps], [stg]))
                self.act(dstF[:, :], stg[:, 0:T], AF.Identity, [stg, self.vec[l]], [dstF],
                         scale=self.vcol(l, "fcw2", cidx), bias=self.vcol(l, "fcb", cidx))
                self.stt(dstF[:, 1:T], stg[:, 0:T - 1], self.vcol(l, "fcw1", cidx), dstF[:, 1:T], ALU.mult, ALU.add,
                         [stg, dstF, self.vec[l]], [dstF])
                self.stt(dstF[:, 2:T], stg[:, 0:T - 2], self.vcol(l, "fcw0", cidx), dstF[:, 2:T], ALU.mult, ALU.add,
                         [stg, dstF, self.vec[l]], [dstF])
            self.act(cg[:, :], cg[:, :], AF.Silu, [cg], [cg])
            hs = HS[j % 9]
            self.tt(hs[:, :], cg[:, :], cu[:, :], ALU.mult, [cg, cu], [hs])

        def down_group(G):
            ng = len(G)
            for dt in range(8):
                src = self.dr["w_down"][l, G[0] * 128:(G[0] + ng) * 128, dt * 128:(dt + 1) * 128].rearrange("(k p) m -> p k m", p=128)
                wb, wv = self.wload(src, ng, 128)
                for tb in range(4):
                    sl = slice(tb * 512, (tb + 1) * 512)
                    ps = self.nps()
                    for gi, j in enumerate(G):
                        hs = HS[j % 9]
                        self.mm(ps[:, :], wv[:, gi, :], hs[:, sl], gi == 0, gi == ng - 1, [wb, hs], [ps])
                    self.tt(FS[dt][:, sl], ps[:, :], FS[dt][:, sl], ALU.add, [ps, FS[dt]], [FS[dt]])

        prev = None
        for G in groups:
            for gi, j in enumerate(G):
                up_tile(j)
                if gi == 1 and prev is not None:
                    down_group(prev)
                    prev = None
            prev = G
        down_group(prev)

    def layer_consts(self, l):
        P = self.P
        sm = self.small
        self.memset(sm[:, 1:2], EPS, [sm])
        self.memset(sm[:, 2:3], 1e-12, [sm])
        self.memset(sm[:, 3:4], 64e-5, [sm])
        for i in range(2):
            if l == 0:
                self.memset(sm[:, 4 + i:5 + i], 0.0, [sm])
                self.memset(sm[:, 6 + i:7 + i], 1.0, [sm])
            else:
                self.tt(sm[:, 10 + i:11 + i], self.vcol(l, "lb1", i), self.vcol(l, "lb0", i), ALU.subtract, [self.vec[l]], [sm])
                self.act(sm[:, 4 + i:5 + i], sm[:, 10 + i:11 + i], AF.Sigmoid, [sm], [sm])
                self.ts(sm[:, 6 + i:7 + i], sm[:, 4 + i:5 + i], -1.0, 1.0, ALU.mult, ALU.add, [sm], [sm])
            self.ts(sm[:, 8 + i:9 + i], self.vcol(l, "f_b", i), -1.0, None, ALU.mult, None, [self.vec[l]], [sm])
        P.dma("pool", self.misc[:, :], self.dr["miscw"][l], writes=[self.misc])

    def run(self, stages):
        P = self.P
        self.setup()
        self.nrot = 8
        self.load_x()
        di = 0
        for l in range(DEPTH):
            self.layer_consts(l)
            self.rmsnorm(l, "nmg")
            for kc in range(8):
                P.dma("sp", self.dr["hscr"][kc], self.FS[kc][:, :], reads=[self.FS[kc]], writes=[self.hb[kc]])
            if l == 0:
                self.nrot = 6
                self.psi = self.psi % 6
            if "A" in stages:
                self.rwkv(l)
            else:
                for t_ in self.yA:
                    self.memset(t_[:, :], 0.0, [t_])
            if "B" in stages:
                self.attn(l)
            if "C" in stages:
                self.mlstm(l)
            if "D" in stages:
                self.hgrn2(l)
            if self.dbg is not None and l == 0:
                for t_ in self.yA + self.yB + self.yC + self.yD:
                    self.dump(di, t_, t_[:, :])
                    di += 1
            if "M" not in stages:
                break
            self.nrot = 8
            self.merge_wout(l)
            self.rmsnorm(l, "nfg")
            self.ffn(l)
            if l + 1 < DEPTH:
                self.nrot = 6
                self.psi = self.psi % 6
        if "M" in stages:
            toks = []

            ones = self.cst["ones"]
            for tb in range(4):
                sl = slice(tb * 512, (tb + 1) * 512)
                ps = self.nps()
                for kc in range(8):
                    sq = self.tb()
                    self.act(sq[:, :], self.FS[kc][:, sl], AF.Square, [self.FS[kc]], [sq])
                    self.mm(ps[:, :], ones[:, :], sq[:, :], kc == 0, kc == 7, [ones, sq], [ps])
                lnv = self.ta()
                self.act(lnv[:, :], ps[:, :], AF.Ln, [ps, self.small], [lnv], scale=1.0 / D, bias=self.small[:, 1:2])
                rstd = TT(self.pcx.t[:, 0:512], self.pcx.b)
                self.act(rstd[:, :], lnv[:, :], AF.Exp, [lnv], [rstd], scale=-0.5)
                for kc in range(8):
                    self.stt(self.FS[kc][:, sl], self.FS[kc][:, sl], self.vcol(0, "fing", kc), rstd[:, :],
                             ALU.mult, ALU.mult, [self.FS[kc], rstd, self.vec[0]], [self.FS[kc]])
                for j in range(4):
                    stg = self.HS[j % 2]
                    stgv = stg.t[:, :].bitcast(F32)
                    cs_ = slice(tb * 512 + j * 128, tb * 512 + (j + 1) * 128)
                    for half in range(2):
                        pst = self.nps()
                        for q4 in range(4):
                            kc = half * 4 + q4
                            self.tr(pst[:, q4 * 128:(q4 + 1) * 128], self.FS[kc][:, cs_], self.identF[:, :],
                                    [self.FS[kc], self.identF], [pst])
                        self.cp(stgv[:, half * 512:(half + 1) * 512], pst[:, :], [pst], [stg],
                                eng=("act" if half == 0 else "dve"))
                    r0 = tb * 512 + j * 128
                    toks.append(P.dma("sp", self.dr["out"][r0:r0 + 128, :], stgv, reads=[stg]))
            P.finish_wait("sp", toks)
        if self.dbg_toks:
            P.finish_wait("pool", self.dbg_toks)


def build(stages="ABCDM", debug=False):
    nc = bass.Bass("TRN2", target_bir_lowering=False)
    dr = {}
    dr["x"] = nc.dram_tensor("x", [T, D], F32, kind="ExternalInput").ap()
    dr["w_in"] = nc.dram_tensor("w_in", [DEPTH, D, NWT * 128], F32, kind="ExternalInput").ap()
    dr["vecs"] = nc.dram_tensor("vecs", [DEPTH, 128, NVEC], F32, kind="ExternalInput").ap()
    dr["cst"] = nc.dram_tensor("cst", [128, NCST], F32, kind="ExternalInput").ap()
    dr["miscw"] = nc.dram_tensor("miscw", [DEPTH, 128, 256], F32, kind="ExternalInput").ap()
    dr["pall"] = nc.dram_tensor("pall", [DEPTH, 896, D], F32, kind="ExternalInput").ap()
    dr["w_out"] = nc.dram_tensor("w_out", [DEPTH, D, D], F32, kind="ExternalInput").ap()
    dr["w_up"] = nc.dram_tensor("w_up", [DEPTH, D, 2 * DFF], F32, kind="ExternalInput").ap()
    dr["w_down"] = nc.dram_tensor("w_down", [DEPTH, DFF, D], F32, kind="ExternalInput").ap()
    dr["out"] = nc.dram_tensor("out", [T, D], F32, kind="ExternalOutput").ap()
    dr["hscr"] = nc.dram_tensor("hscr", [8, 128, T], F32, kind="Internal").ap()
    dbg = None
    if debug:
        dbg = nc.dram_tensor("dbg", [8, 128, T], F32, kind="ExternalOutput").ap()
    with ExitStack() as es:
        P = Prog(nc, es)
        K = Kern(nc, P, dr, dbg)
        K.run(stages)
        P.emit()
        print(f"[build] ops={P.n_ops} waits={P.n_wait} counts={P.count}")
    return nc


def host_inputs(inp):
    inp = {k: np.asarray(v) for k, v in inp.items()}
    shared = {}
    shared["w_in"] = np.ascontiguousarray(inp["w_in"][:, :, WIN_COLS])
    shared["vecs"] = np.stack([_pack_vecs(inp, l) for l in range(DEPTH)])
    shared["cst"] = _consts()
    shared["miscw"] = np.ascontiguousarray(np.concatenate([inp["rwkv_w2"], inp["rwkv_a2"], inp["rwkv_g2"]], axis=1))
    shared["pall"] = np.ascontiguousarray(np.concatenate([inp["p_rwkv"], inp["p_attn"], inp["p_mlstm"], inp["p_hgrn"]], axis=1))
    shared["w_out"] = np.ascontiguousarray(inp["w_out"])
    shared["w_up"] = np.ascontiguousarray(inp["w_up"])
    shared["w_down"] = np.ascontiguousarray(inp["w_down"])
    return shared


_NC_CACHE = {}


def kernel(**inputs):
    shared = host_inputs(inputs)
    x = np.asarray(inputs["x"], np.float32)
    if "nc" not in _NC_CACHE:
        _NC_CACHE["nc"] = build("ABCDM", False)
    nc = _NC_CACHE["nc"]
    in_maps = []
    for c in range(8):
        m = dict(shared)
        m["x"] = np.ascontiguousarray(x[c])
        in_maps.append(m)
    res = run_bass_kernel_spmd(nc, in_maps, core_ids=list(range(8)))
    return np.stack([np.asarray(r["out"], np.float32) for r in res.results], axis=0)
```
